# Optimizing a Trainium2 kernel written in Bass

```python
import math
import jax
import jax.numpy as jnp
from jax import lax
import numpy as np

D_MODEL = 1024
BATCH = 8
SEQ = 2048
DEPTH = 4

GRID_W = 64
CTX_LEN = 256
NORM_EPS = 1e-6
N_MOD = 6

CONV_CH = 512
CONV_W = 3
MLA_HEADS = 8
Q_RANK = 256
KV_RANK = 128
QK_NOPE = 64
QK_ROPE = 32
V_HEAD = 64
MLA_SCALE = (QK_NOPE + QK_ROPE) ** -0.5
ROPE_BASE = 10000.0
Q_BLOCK = 128
EVEN_IN = 3 * CONV_CH + Q_RANK + KV_RANK + QK_ROPE
EVEN_MIX = CONV_CH + MLA_HEADS * V_HEAD
EVEN_SPLITS = (CONV_CH, 2 * CONV_CH, 3 * CONV_CH, 3 * CONV_CH + Q_RANK, 3 * CONV_CH + Q_RANK + KV_RANK)

GDN_HEADS = 8
GDN_DK = 128
GDN_DV = 128
GDN_CONV_W = 3
GDN_CHUNK = 64
GDN_QK = GDN_HEADS * GDN_DK
GDN_V = GDN_HEADS * GDN_DV
ODD_IN = 2 * GDN_QK + 2 * GDN_V + 4 * GDN_HEADS
ODD_SPLITS = (2 * GDN_QK + GDN_V, 2 * GDN_QK + 2 * GDN_V, 2 * GDN_QK + 2 * GDN_V + 2 * GDN_HEADS)

FFN_DENSE = 2816
N_EXPERTS = 8
TOP_K = 2
FFN_EXPERT = 3584

kernel_name = "hybrid_conv_mla_gdn_moe_diffusion_trunk"


def rmsnorm(x, g):
    xf = x.astype(jnp.float32)
    y = xf * lax.rsqrt(jnp.mean(xf * xf, axis=-1, keepdims=True) + NORM_EPS)
    return (y * g.astype(jnp.float32)).astype(x.dtype)


def l2norm(x):
    return x * lax.rsqrt(jnp.sum(x * x, axis=-1, keepdims=True) + NORM_EPS)


def adaln_params(cond, mod_w, mod_b):
    m = (jax.nn.silu(cond) @ mod_w + mod_b)[..., None, :]
    return jnp.split(m, N_MOD, axis=-1)


def modulate(h, shift, scale):
    return h * (1.0 + scale) + shift


def dwconv_centred(x, w):
    k_w = w.shape[0]
    pad = k_w // 2
    L = x.shape[1]
    xp = jnp.pad(x, ((0, 0), (pad, pad), (0, 0)))
    y = xp[:, 0:L] * w[0]
    for j in range(1, k_w):
        y = y + xp[:, j:j + L] * w[j]
    return y


def axial_rope_tables(row, col, dtype):
    n = QK_ROPE // 4
    inv = ROPE_BASE ** (-jnp.arange(n, dtype=jnp.float32) / n)
    ang = jnp.concatenate([row.astype(jnp.float32)[:, None] * inv, col.astype(jnp.float32)[:, None] * inv], axis=-1)
    return jnp.cos(ang).astype(dtype), jnp.sin(ang).astype(dtype)


def apply_axial_rope(x, cos, sin):
    q = x.shape[-1] // 4
    x1, x2, x3, x4 = jnp.split(x, 4, axis=-1)
    a = jnp.concatenate([x1, x3], axis=-1)
    b = jnp.concatenate([x2, x4], axis=-1)
    ra = a * cos - b * sin
    rb = b * cos + a * sin
    return jnp.concatenate([ra[..., :q], rb[..., :q], ra[..., q:], rb[..., q:]], axis=-1)


def mla_attend(q_nope, q_rope, k_nope, k_rope, v):
    s = jnp.einsum("bqhd,bkhd->bhqk", q_nope, k_nope) + jnp.einsum("bqhr,bkr->bhqk", q_rope, k_rope)
    p = jax.nn.softmax(s.astype(jnp.float32) * MLA_SCALE, axis=-1).astype(v.dtype)
    return jnp.einsum("bhqk,bkhd->bqhd", p, v)


def latent_attention(q_nope, q_rope, k_nope, k_rope, v):
    b, L = q_nope.shape[:2]
    nb = L // Q_BLOCK

    def blocks(t):
        return jnp.moveaxis(t.reshape(b, nb, Q_BLOCK, *t.shape[2:]), 1, 0)

    out = lax.map(lambda qs: mla_attend(qs[0], qs[1], k_nope, k_rope, v), (blocks(q_nope), blocks(q_rope)))
    return jnp.moveaxis(out, 0, 1).reshape(b, L, MLA_HEADS * V_HEAD)


def unit_lower_inverse(lm):
    eye = jnp.eye(lm.shape[-1], dtype=lm.dtype)
    p = -lm
    t = eye + p
    for _ in range((lm.shape[-1] - 1).bit_length() - 1):
        p = p @ p
        t = t @ (eye + p)
    return t


def gated_delta_chunked(q, k, v, g, beta, state):
    b, L, h, _ = k.shape
    dv = v.shape[-1]
    n = L // GDN_CHUNK

    def chunks(t):
        return jnp.swapaxes(t.reshape(b, n, GDN_CHUNK, *t.shape[2:]), 2, 3)

    q, k, v, g, beta = (chunks(t) for t in (q, k, v, g, beta))
    gcum = jnp.cumsum(g, axis=-1)
    incl = jnp.tril(jnp.ones((GDN_CHUNK, GDN_CHUNK), dtype=bool))
    decay = jnp.exp(jnp.where(incl, gcum[..., :, None] - gcum[..., None, :], -jnp.inf))
    kb = k * beta[..., None]
    strict = jnp.tril(jnp.einsum("bnhid,bnhjd->bnhij", kb, k) * decay, -1)
    tinv = unit_lower_inverse(strict)
    u = tinv @ (v * beta[..., None])
    w = tinv @ (kb * jnp.exp(gcum)[..., None])
    attn = jnp.einsum("bnhid,bnhjd->bnhij", q, k) * decay
    qg = q * jnp.exp(gcum)[..., None]
    kd = k * jnp.exp(gcum[..., -1:] - gcum)[..., None]
    gend = jnp.exp(gcum[..., -1])[..., None, None]

    def step(s, xs):
        u_i, w_i, a_i, qg_i, kd_i, ge_i = xs
        v_new = u_i - w_i @ s
        o_i = qg_i @ s + a_i @ v_new
        s = s * ge_i + jnp.swapaxes(kd_i, -1, -2) @ v_new
        return s, o_i

    xs = tuple(jnp.moveaxis(t, 1, 0) for t in (u, w, attn, qg, kd, gend))
    state, o = lax.scan(step, state, xs)
    o = jnp.swapaxes(jnp.moveaxis(o, 0, 1), 2, 3).reshape(b, L, h, dv)
    return o, state


def scan_direction(q, k, v, g, beta, state, reverse):
    if reverse:
        o, s = gated_delta_chunked(*(jnp.flip(t, 1) for t in (q, k, v, g, beta)), state)
        return jnp.flip(o, 1), s
    return gated_delta_chunked(q, k, v, g, beta, state)


def swiglu(h, w1, w2):
    gate, up = jnp.split(h @ w1, 2, axis=-1)
    return (jax.nn.silu(gate) * up) @ w2


def moe_swiglu(h, router_w, w1, w2):
    logits = (h @ router_w).astype(jnp.float32)
    top_v, top_i = lax.top_k(logits, TOP_K)
    top_w = jax.nn.softmax(top_v, axis=-1)
    gates = jnp.sum(jax.nn.one_hot(top_i, N_EXPERTS, dtype=jnp.float32) * top_w[..., None], axis=-2).astype(h.dtype)
    y = gates[..., 0:1] * swiglu(h, w1[0], w2[0])
    for e in range(1, N_EXPERTS):
        y = y + gates[..., e:e + 1] * swiglu(h, w1[e], w2[e])
    return y


def even_mixer(hc, hx, cos, sin, w_in, conv_w, q_norm_g, w_uq, kv_norm_g, w_ukv, w_out, ctx_out):
    def project(h):
        b, L = h.shape[:2]
        gb, gc, u, cq, ckv, kr = jnp.split(h @ w_in, EVEN_SPLITS, axis=-1)
        conv_y = gb * dwconv_centred(gc * u, conv_w)
        q = (rmsnorm(cq, q_norm_g) @ w_uq).reshape(b, L, MLA_HEADS, QK_NOPE + QK_ROPE)
        kv = (rmsnorm(ckv, kv_norm_g) @ w_ukv).reshape(b, L, MLA_HEADS, QK_NOPE + V_HEAD)
        return conv_y, q[..., :QK_NOPE], q[..., QK_NOPE:], kv[..., :QK_NOPE], kr, kv[..., QK_NOPE:]

    cy_c, qn_c, qr_c, kn_c, kr_c, v_c = project(hc)
    cy_x, qn_x, qr_x, kn_x, kr_x, v_x = project(hx)
    qr_x = apply_axial_rope(qr_x, cos[:, None, :], sin[:, None, :])
    kr_x = apply_axial_rope(kr_x, cos, sin)
    k_nope = jnp.concatenate([kn_c, kn_x], axis=1)
    k_rope = jnp.concatenate([kr_c, kr_x], axis=1)
    v = jnp.concatenate([v_c, v_x], axis=1)
    att_x = latent_attention(qn_x, qr_x, k_nope, k_rope, v)
    out_x = jnp.concatenate([cy_x, att_x], axis=-1) @ w_out
    out_c = None
    if ctx_out:
        b, lc = hc.shape[:2]
        att_c = mla_attend(qn_c, qr_c, kn_c, kr_c, v_c).reshape(b, lc, MLA_HEADS * V_HEAD)
        out_c = jnp.concatenate([cy_c, att_c], axis=-1) @ w_out
    return out_c, out_x


def odd_mixer(hc, hx, w_in, qkv_conv_w, a_log, dt_bias, o_norm_g, w_out, ctx_out):
    def project(h):
        b, L = h.shape[:2]
        qkv, gate, a, bt = jnp.split(h @ w_in, ODD_SPLITS, axis=-1)
        qkv = jax.nn.silu(dwconv_centred(qkv, qkv_conv_w)).astype(jnp.float32)
        q, k, v = jnp.split(qkv, (GDN_QK, 2 * GDN_QK), axis=-1)
        q = l2norm(q.reshape(b, L, GDN_HEADS, GDN_DK)) * (GDN_DK ** -0.5)
        k = l2norm(k.reshape(b, L, GDN_HEADS, GDN_DK))
        v = v.reshape(b, L, GDN_HEADS, GDN_DV)
        a = a.astype(jnp.float32).reshape(b, L, 2, GDN_HEADS)
        g = -jnp.exp(a_log.astype(jnp.float32)) * jax.nn.softplus(a + dt_bias.astype(jnp.float32))
        beta = jax.nn.sigmoid(bt.astype(jnp.float32).reshape(b, L, 2, GDN_HEADS))
        return q, k, v, g, beta, gate.reshape(b, L, GDN_HEADS, GDN_DV)

    def output(o, gate, dtype):
        b, L = o.shape[:2]
        y = rmsnorm(o, o_norm_g) * jax.nn.silu(gate.astype(jnp.float32))
        return y.reshape(b, L, GDN_V).astype(dtype) @ w_out

    qc, kc, vc, gcc, bc, zc = project(hc)
    qx, kx, vx, gx, bx, zx = project(hx)
    s0 = jnp.zeros((hx.shape[0], GDN_HEADS, GDN_DK, GDN_DV), jnp.float32)
    oc_f, sc_f = scan_direction(qc, kc, vc, gcc[:, :, 0], bc[:, :, 0], s0, False)
    oc_b, sc_b = scan_direction(qc, kc, vc, gcc[:, :, 1], bc[:, :, 1], s0, True)
    ox_f, _ = scan_direction(qx, kx, vx, gx[:, :, 0], bx[:, :, 0], sc_f, False)
    ox_b, _ = scan_direction(qx, kx, vx, gx[:, :, 1], bx[:, :, 1], sc_b, True)
    out_x = output(ox_f + ox_b, zx, hx.dtype)
    out_c = output(oc_f + oc_b, zc, hc.dtype) if ctx_out else None
    return out_c, out_x


def trunk_layer(x, ctx, c, c_ctx, mod_w, mod_b, norm1_g, norm2_g, mixer, ffn, ctx_out):
    sx1, ax1, gx1, sx2, ax2, gx2 = adaln_params(c, mod_w, mod_b)
    sc1, ac1, gc1, sc2, ac2, gc2 = adaln_params(c_ctx, mod_w, mod_b)
    hx = modulate(rmsnorm(x, norm1_g), sx1, ax1)
    hc = modulate(rmsnorm(ctx, norm1_g), sc1, ac1)
    oc, ox = mixer(hc, hx)
    x = x + gx1 * ox
    hx = modulate(rmsnorm(x, norm2_g), sx2, ax2)
    if ctx_out:
        ctx = ctx + gc1 * oc
        hc = modulate(rmsnorm(ctx, norm2_g), sc2, ac2)
        lc = ctx.shape[1]
        y = ffn(jnp.concatenate([hc, hx], axis=1))
        ctx = ctx + gc2 * y[:, :lc]
        x = x + gx2 * y[:, lc:]
    else:
        x = x + gx2 * ffn(hx)
    return x, ctx


def even_layer(x, ctx, c, c_ctx, cos, sin, mod_w, mod_b, norm1_g, w_in, conv_w, q_norm_g, w_uq, kv_norm_g,
               w_ukv, w_out, norm2_g, ffn_w1, ffn_w2, ctx_out):
    mixer = lambda hc, hx: even_mixer(hc, hx, cos, sin, w_in, conv_w, q_norm_g, w_uq, kv_norm_g, w_ukv, w_out, ctx_out)
    ffn = lambda h: swiglu(h, ffn_w1, ffn_w2)
    return trunk_layer(x, ctx, c, c_ctx, mod_w, mod_b, norm1_g, norm2_g, mixer, ffn, ctx_out)


def odd_layer(x, ctx, c, c_ctx, mod_w, mod_b, norm1_g, w_in, qkv_conv_w, a_log, dt_bias, o_norm_g, w_out,
              norm2_g, router_w, moe_w1, moe_w2, ctx_out):
    mixer = lambda hc, hx: odd_mixer(hc, hx, w_in, qkv_conv_w, a_log, dt_bias, o_norm_g, w_out, ctx_out)
    ffn = lambda h: moe_swiglu(h, router_w, moe_w1, moe_w2)
    return trunk_layer(x, ctx, c, c_ctx, mod_w, mod_b, norm1_g, norm2_g, mixer, ffn, ctx_out)


def setup_inputs(seed: int = 0) -> dict:
    base = jax.random.key(seed)
    counter = iter(range(100000))

    def nk():
        return jax.random.fold_in(base, next(counter))

    def normal(shape, scale):
        return scale * jax.random.normal(nk(), shape, jnp.float32)

    def gain(n):
        return 1.0 + normal((n,), 0.02)

    d = D_MODEL
    inp = {}
    inp["x"] = normal((BATCH, SEQ, d), 1.0)
    inp["c"] = normal((BATCH, d), 1.0)
    inp["ctx"] = normal((BATCH, CTX_LEN, d), 1.0)
    inp["c_ctx"] = normal((d,), 1.0)
    for i in range(DEPTH):
        p = "l%d_" % i
        inp[p + "mod_w"] = normal((d, N_MOD * d), 0.5 * d ** -0.5)
        inp[p + "mod_b"] = normal((N_MOD * d,), 0.02)
        inp[p + "norm1_g"] = gain(d)
        if i % 2 == 0:
            inp[p + "w_in"] = normal((d, EVEN_IN), d ** -0.5)
            inp[p + "conv_w"] = normal((CONV_W, CONV_CH), CONV_W ** -0.5)
            inp[p + "q_norm_g"] = gain(Q_RANK)
            inp[p + "w_uq"] = normal((Q_RANK, MLA_HEADS * (QK_NOPE + QK_ROPE)), Q_RANK ** -0.5)
            inp[p + "kv_norm_g"] = gain(KV_RANK)
            inp[p + "w_ukv"] = normal((KV_RANK, MLA_HEADS * (QK_NOPE + V_HEAD)), KV_RANK ** -0.5)
            inp[p + "w_out"] = normal((EVEN_MIX, d), EVEN_MIX ** -0.5)
            inp[p + "norm2_g"] = gain(d)
            inp[p + "ffn_w1"] = normal((d, 2 * FFN_DENSE), d ** -0.5)
            inp[p + "ffn_w2"] = normal((FFN_DENSE, d), FFN_DENSE ** -0.5)
        else:
            inp[p + "w_in"] = normal((d, ODD_IN), d ** -0.5)
            inp[p + "qkv_conv_w"] = normal((GDN_CONV_W, 2 * GDN_QK + GDN_V), GDN_CONV_W ** -0.5)
            inp[p + "a_log"] = jnp.log(jax.random.uniform(nk(), (2, GDN_HEADS), jnp.float32, 1.0, 16.0))
            dt = jnp.exp(jax.random.uniform(nk(), (2, GDN_HEADS), jnp.float32, math.log(1e-3), math.log(1e-1)))
            inp[p + "dt_bias"] = dt + jnp.log(-jnp.expm1(-dt))
            inp[p + "o_norm_g"] = gain(GDN_DV)
            inp[p + "w_out"] = normal((GDN_V, d), GDN_V ** -0.5)
            inp[p + "norm2_g"] = gain(d)
            inp[p + "router_w"] = normal((d, N_EXPERTS), d ** -0.5)
            inp[p + "moe_w1"] = normal((N_EXPERTS, d, 2 * FFN_EXPERT), d ** -0.5)
            inp[p + "moe_w2"] = normal((N_EXPERTS, FFN_EXPERT, d), FFN_EXPERT ** -0.5)
    inp["final_norm_g"] = gain(d)
    return inp


def reference(x, c, ctx, c_ctx,
              l0_mod_w, l0_mod_b, l0_norm1_g, l0_w_in, l0_conv_w, l0_q_norm_g, l0_w_uq, l0_kv_norm_g, l0_w_ukv,
              l0_w_out, l0_norm2_g, l0_ffn_w1, l0_ffn_w2,
              l1_mod_w, l1_mod_b, l1_norm1_g, l1_w_in, l1_qkv_conv_w, l1_a_log, l1_dt_bias, l1_o_norm_g,
              l1_w_out, l1_norm2_g, l1_router_w, l1_moe_w1, l1_moe_w2,
              l2_mod_w, l2_mod_b, l2_norm1_g, l2_w_in, l2_conv_w, l2_q_norm_g, l2_w_uq, l2_kv_norm_g, l2_w_ukv,
              l2_w_out, l2_norm2_g, l2_ffn_w1, l2_ffn_w2,
              l3_mod_w, l3_mod_b, l3_norm1_g, l3_w_in, l3_qkv_conv_w, l3_a_log, l3_dt_bias, l3_o_norm_g,
              l3_w_out, l3_norm2_g, l3_router_w, l3_moe_w1, l3_moe_w2,
              final_norm_g):
    rows = x.shape[1] // GRID_W
    row = jnp.repeat(jnp.arange(rows), GRID_W)
    col = jnp.tile(jnp.arange(GRID_W), rows)
    cos, sin = axial_rope_tables(row, col, x.dtype)
    layers = (
        (l0_mod_w, l0_mod_b, l0_norm1_g, l0_w_in, l0_conv_w, l0_q_norm_g, l0_w_uq, l0_kv_norm_g, l0_w_ukv,
         l0_w_out, l0_norm2_g, l0_ffn_w1, l0_ffn_w2),
        (l1_mod_w, l1_mod_b, l1_norm1_g, l1_w_in, l1_qkv_conv_w, l1_a_log, l1_dt_bias, l1_o_norm_g,
         l1_w_out, l1_norm2_g, l1_router_w, l1_moe_w1, l1_moe_w2),
        (l2_mod_w, l2_mod_b, l2_norm1_g, l2_w_in, l2_conv_w, l2_q_norm_g, l2_w_uq, l2_kv_norm_g, l2_w_ukv,
         l2_w_out, l2_norm2_g, l2_ffn_w1, l2_ffn_w2),
        (l3_mod_w, l3_mod_b, l3_norm1_g, l3_w_in, l3_qkv_conv_w, l3_a_log, l3_dt_bias, l3_o_norm_g,
         l3_w_out, l3_norm2_g, l3_router_w, l3_moe_w1, l3_moe_w2),
    )
    for i in range(DEPTH):
        ctx_out = i < DEPTH - 1
        if i % 2 == 0:
            x, ctx = even_layer(x, ctx, c, c_ctx, cos, sin, *layers[i], ctx_out=ctx_out)
        else:
            x, ctx = odd_layer(x, ctx, c, c_ctx, *layers[i], ctx_out=ctx_out)
    return rmsnorm(x, final_norm_g)
```

```python
import contextlib
import numpy as np
import concourse.bass as bass
import concourse.mybir as mybir
from concourse.bass_utils import run_bass_kernel_spmd

F32 = mybir.dt.float32
BF16 = mybir.dt.bfloat16
AF = mybir.ActivationFunctionType
ALU = mybir.AluOpType
AX = mybir.AxisListType

ENGS = ("pe", "act", "dve", "pool", "sp")
NT = 2304
NCTX = 256
EPS = 1e-6


class _Rec:
    def __init__(self):
        self.call = None

    def __getattr__(self, name):
        def f(*a, **kw):
            self.call = (name, a, kw)
            return self
        return f


class Prog:
    def __init__(self, nc, same_engine_sync=True):
        self.nc = nc
        self.q = {e: [] for e in ENGS}
        self.cnt = {e: 0 for e in ENGS}
        self.last_w = {}
        self.readers = {}
        self.dma_sems = {}
        self.last_tok = {}
        self.same_engine_sync = same_engine_sync
        self.btoks = []
        self.bar_fns = {}

    def op(self, eng, fn, reads=(), writes=(), dma=None, extra=()):
        deps = set(extra) | set(self.btoks)
        for k in reads:
            t = self.last_w.get(k)
            if t is not None:
                deps.add(t)
        for k in writes:
            t = self.last_w.get(k)
            if t is not None:
                deps.add(t)
            for r in self.readers.get(k, ()):
                deps.add(r)
        if dma is None:
            self.cnt[eng] += 1
            tok = (eng, self.cnt[eng])
        else:
            self.dma_sems[dma] = self.dma_sems.get(dma, 0) + 16
            tok = ("dma:" + dma, self.dma_sems[dma])
        r = _Rec()
        fn(r)
        self.q[eng].append((r.call, deps, tok))
        self.last_tok[tok[0]] = tok
        for k in reads:
            self.readers.setdefault(k, []).append(tok)
        for k in writes:
            self.last_w[k] = tok
            self.readers[k] = []
        return tok

    def pe(self, fn, reads=(), writes=()):
        return self.op("pe", fn, reads, writes)

    def act(self, fn, reads=(), writes=()):
        return self.op("act", fn, reads, writes)

    def dve(self, fn, reads=(), writes=()):
        return self.op("dve", fn, reads, writes)

    def dma(self, eng, slot, fn, reads=(), writes=()):
        return self.op(eng, fn, reads, writes, dma=slot)

    def barrier(self):
        toks = list(self.last_tok.values()) + list(self.btoks)
        nb = []
        for e in ("pe", "act", "dve", "pool"):
            nb.append(self.op(e, self.bar_fns[e], extra=toks))
        self.btoks = nb
        self.last_w = {}
        self.readers = {}

    def emit(self, final_waits=()):
        nc = self.nc
        sem_names = list(ENGS) + ["dma:" + s for s in self.dma_sems]
        with contextlib.ExitStack() as es:
            sems = {}
            for i, n in enumerate(sem_names):
                sems[n] = es.enter_context(nc.semaphore("s%d" % i))
            block = es.enter_context(nc.Block())
            handles = {"pe": "tensor", "act": "scalar", "dve": "vector", "pool": "gpsimd", "sp": "sync"}

            def make(engname):
                ops = self.q[engname]

                def body(eng):
                    seen = {}
                    for (fn, deps, tok) in ops:
                        need = {}
                        for (s, v) in deps:
                            if s == engname and (engname == "pe" or not self.same_engine_sync):
                                continue
                            if seen.get(s, 0) < v:
                                need[s] = max(need.get(s, 0), v)
                        for s, v in need.items():
                            eng.wait_ge(sems[s], v)
                            seen[s] = v
                        ins = getattr(eng, fn[0])(*fn[1], **fn[2])
                        ins.then_inc(sems[tok[0]], 16 if tok[0].startswith("dma:") else 1)
                    if engname == "sp":
                        for tok in final_waits:
                            eng.wait_ge(sems[tok[0]], tok[1])
                return body

            for engname in ENGS:
                if not self.q[engname] and engname != "sp":
                    continue
                getattr(block, handles[engname])(make(engname))


def blocks_of(start, length, maxw=512):
    out = []
    n = (length + maxw - 1) // maxw
    w = length // n
    assert w * n == length
    for i in range(n):
        out.append((start + i * w, w))
    return out


EVEN_IN = 1952
_DBG = {}


class _Stop(Exception):
    pass


def _step(name):
    lim = _DBG.get("astep")
    if lim is None:
        return
    _DBG["_cnt"] = _DBG.get("_cnt", 0) + 1
    if _DBG["_cnt"] >= lim:
        print("STOP at step", _DBG["_cnt"], name)
        raise _Stop()
ODD_IN = 4128


def build(layers=(0, 1, 2, 3), final=True):
    nc = bass.Bass("TRN2", target_bir_lowering=False)
    D = {}

    def din(name, shape, dt=F32):
        D[name] = nc.dram_tensor(name, list(shape), dt, kind="ExternalInput").ap()
        return D[name]

    din("xT", [128, 8, NT]); din("csT", [128, 8, 2])
    din("ident", [128, 128]); din("masks", [128, 6, 128]); din("rope", [96, 2, NT])
    din("final_g", [128, 8])
    for l in layers:
        p = "l%d_" % l
        din(p + "mod_w", [1024, 6144]); din(p + "mod_b", [128, 48]); din(p + "n1g", [128, 8]); din(p + "n2g", [128, 8])
        din(p + "w_out", [1024, 1024])
        if l % 2 == 0:
            din(p + "w_in", [1024, EVEN_IN]); din(p + "wkr", [1024, 2, 96]); din(p + "convw", [128, 4, 3])
            din(p + "qng", [128, 2]); din(p + "kvng", [128, 1]); din(p + "w_uq", [256, 2, 768])
            din(p + "w_uk", [128, 8, 96]); din(p + "w_uv", [128, 512])
            din(p + "ffn_w1", [1024, 5632]); din(p + "ffn_w2", [2816, 1024])
        else:
            din(p + "w_in", [1024, ODD_IN]); din(p + "convw", [128, 24, 3]); din(p + "alog", [128, 16]); din(p + "dtb", [128, 16])
            din(p + "ong", [128, 128]); din(p + "router", [1024, 8])
            din(p + "moe_w1", [8, 1024, 7168]); din(p + "moe_w2", [8, 3584, 1024])
    if final:
        out_d = nc.dram_tensor("outT", [128, 8, 2048], F32, kind="ExternalOutput").ap()
    else:
        out_d = nc.dram_tensor("outT", [128, 8, NT], F32, kind="ExternalOutput").ap()

    es = contextlib.ExitStack()
    with es:
        def sb(name, shape, dt):
            return es.enter_context(nc.sbuf_tensor(name, list(shape), dt))
        XT = sb("XT", [128, 8, NT], F32)
        HT = sb("HT", [128, 8, NT], BF16)
        ARB = 92 * 1024
        AR = sb("AR", [128, ARB // 4], F32)
        identf = sb("identf", [128, 128], F32)
        identb = sb("identb", [128, 128], BF16)
        scr = sb("scr", [128, 4], F32)
        onesf = sb("onesf", [128, 128], F32)
        onesb = sb("onesb", [128, 128], BF16)
        epsb = sb("epsb", [128, 1], F32)
        sc = sb("sc", [128, 8, 2], F32)
        macc = sb("macc", [128, 48, 2], F32)
        modb = sb("modb", [128, 48], F32)
        ng = sb("ng", [128, 2, 8], F32)
        AB = sb("ABm", [128, 4, 8, 2], F32)
        smallp = sb("smallp", [128, 64], F32)
        PS = [es.enter_context(nc.psum_tensor("ps%d" % i, [128, 512], F32)) for i in range(8)]
        P = Prog(nc, same_engine_sync=not _DBG.get("nosame", False))
        P.bar_fns = {
            "pe": lambda e: e.matmul(PS[7][0:1, 0:1], lhsT=identb[:, 0:1], rhs=identb[:, 0:1], start=True, stop=True),
            "act": lambda e: e.activation(out=scr[:, 0:1], in_=epsb[:, 0:1], func=AF.Identity),
            "dve": lambda e: e.memset(scr[:, 1:2], 0.0),
            "pool": lambda e: e.memset(scr[:, 2:3], 0.0),
        }
        psi = [0]

        def bank():
            i = psi[0] % 6
            psi[0] += 1
            return PS[i], "ps%d" % i

        def carve(off, shape, dt):
            n = int(np.prod(shape))
            bpe = 4 if dt == F32 else 2
            nby = n * bpe
            assert off % 4 == 0 and off + nby <= ARB, (off, nby, ARB)
            ap = AR[:, off // 4:(off + nby + 3) // 4]
            if dt != F32:
                ap = ap.bitcast(dt)[:, 0:n]
            if len(shape) == 2:
                ap = ap.rearrange("p (a b) -> p a b", b=shape[1])
            elif len(shape) == 3:
                ap = ap.rearrange("p (a b c) -> p a b c", b=shape[1], c=shape[2])
            return ap, off + ((nby + 3) // 4) * 4

        P.dma("sp", "c0", lambda e: e.dma_start(out=XT[:], in_=D["xT"][:, :, :]), writes=["XT"])
        P.dma("sp", "c1", lambda e: e.dma_start(out=identf[:], in_=D["ident"][:, :]), writes=["identf"])
        P.dma("sp", "c3", lambda e: e.dma_start(out=sc[:], in_=D["csT"][:, :, :]), writes=["sc"])
        P.dve(lambda e: e.memset(onesf[:], 1.0), writes=["onesf"])
        P.dve(lambda e: e.memset(onesb[:], 1.0), writes=["onesb"])
        P.dve(lambda e: e.memset(epsb[:], EPS), writes=["epsb"])
        P.dve(lambda e: e.tensor_copy(out=identb[:], in_=identf[:]), reads=["identf"], writes=["identb"])
        P.act(lambda e: e.activation(out=sc[:], in_=sc[:], func=AF.Silu), reads=["sc"], writes=["sc"])

        CTX = (0, NCTX)
        XS = (NCTX, NT - NCTX)

        def adaln(l):
            p = "l%d_" % l
            P.barrier()
            mw = [carve(0, [6144], F32)[0], carve(6144 * 4, [6144], F32)[0]]
            mwv = D[p + "mod_w"].rearrange("(k p) n -> p k n", p=128)
            P.dma("sp", "modb", lambda e: e.dma_start(out=modb[:], in_=D[p + "mod_b"][:, :]), writes=["modb"])
            P.dma("sp", "ng", lambda e: e.dma_start(out=ng[:, 0, :], in_=D[p + "n1g"][:, :]), writes=["ng0"])
            P.dma("sp", "ng", lambda e: e.dma_start(out=ng[:, 1, :], in_=D[p + "n2g"][:, :]), writes=["ng1"])
            for k in range(8):
                b = mw[k % 2]
                bk = "mw%d" % (k % 2)
                P.dma("sp", bk, lambda e, b=b, k=k: e.dma_start(out=b, in_=mwv[:, k, :]), writes=[bk])
                ps, pk = bank()
                for j in range(48):
                    P.pe(lambda e, j=j, b=b, ps=ps, k=k: e.matmul(ps[:, 2 * j:2 * j + 2], lhsT=b[:, j * 128:(j + 1) * 128],
                                                              rhs=sc[:, k, :], start=True, stop=True),
                         reads=[bk, "sc"], writes=[pk])
                mflat = macc[:].rearrange("p a b -> p (a b)")
                if k == 0:
                    P.dve(lambda e, ps=ps: e.tensor_copy(out=mflat, in_=ps[:, 0:96]), reads=[pk], writes=["macc"])
                else:
                    P.dve(lambda e, ps=ps: e.tensor_tensor(out=mflat, in0=mflat, in1=ps[:, 0:96], op=ALU.add),
                          reads=[pk, "macc"], writes=["macc"])
            P.dve(lambda e: e.tensor_tensor(out=macc[:], in0=macc[:], in1=modb[:, :, None].broadcast_to([128, 48, 2]), op=ALU.add),
                  reads=["macc", "modb"], writes=["macc"])
            for w in range(2):
                sh = macc[:, 24 * w:24 * w + 8, :]
                scl = macc[:, 24 * w + 8:24 * w + 16, :]
                P.dve(lambda e, w=w, scl=scl: e.scalar_tensor_tensor(
                    out=AB[:, 2 * w, :, :], in0=scl, scalar=1.0, in1=ng[:, w, :, None].broadcast_to([128, 8, 2]),
                    op0=ALU.add, op1=ALU.mult), reads=["macc", "ng%d" % w], writes=["AB"])
                P.dve(lambda e, w=w, sh=sh: e.tensor_copy(out=AB[:, 2 * w + 1, :, :], in_=sh), reads=["macc"], writes=["AB"])

        def gate_ap(w, k, col):
            return macc[:, 24 * w + 16 + k, col:col + 1]

        def norm_mod(w, rs_off, tmp_off, h32_cb=None):
            RS, _ = carve(rs_off, [NT], F32)
            T0, o1 = carve(tmp_off, [NT], F32)
            T1, _ = carve(o1, [NT], F32)
            tmps = [T0, T1]
            blks = blocks_of(0, NT - 256, 512) + [(NT - 256, 256)]
            bks = [bank() for _ in blks]
            for k in range(8):
                t = tmps[k % 2]; tk = "nt%d" % (k % 2)
                P.act(lambda e, t=t, k=k: e.activation(out=t, in_=XT[:, k, :], func=AF.Square), reads=["XT"], writes=[tk])
                for (ps, pk), (s, n) in zip(bks, blks):
                    P.pe(lambda e, ps=ps, t=t, s=s, n=n, k=k: e.matmul(ps[:, 0:n], lhsT=onesf[:], rhs=t[:, s:s + n],
                                                                    start=(k == 0), stop=(k == 7)),
                         reads=[tk, "onesf"], writes=[pk])
            for (ps, pk), (s, n) in zip(bks, blks):
                P.act(lambda e, ps=ps, s=s, n=n: e.activation(out=RS[:, s:s + n], in_=ps[:, 0:n], func=AF.Sqrt,
                                                              scale=1.0 / 1024, bias=epsb[:, 0:1]),
                      reads=[pk, "epsb"], writes=["RS"])
            P.dve(lambda e: e.reciprocal(out=RS, in_=RS), reads=["RS"], writes=["RS"])
            for k in range(8):
                t = tmps[k % 2]; tk = "nt%d" % (k % 2)
                P.dve(lambda e, t=t, k=k: e.tensor_tensor(out=t, in0=XT[:, k, :], in1=RS, op=ALU.mult),
                      reads=["XT", "RS"], writes=[tk])
                for col, (s, n) in ((1, CTX), (0, XS)):
                    if h32_cb is not None:
                        P.act(lambda e, t=t, k=k, col=col, s=s, n=n: e.activation(
                            out=t[:, s:s + n], in_=t[:, s:s + n], func=AF.Identity,
                            scale=AB[:, 2 * w, k, col:col + 1], bias=AB[:, 2 * w + 1, k, col:col + 1]),
                            reads=[tk, "AB"], writes=[tk])
                    else:
                        P.act(lambda e, t=t, k=k, col=col, s=s, n=n: e.activation(
                            out=HT[:, k, s:s + n], in_=t[:, s:s + n], func=AF.Identity,
                            scale=AB[:, 2 * w, k, col:col + 1], bias=AB[:, 2 * w + 1, k, col:col + 1]),
                            reads=[tk, "AB"], writes=["HT%d" % k])
                if h32_cb is not None:
                    P.dve(lambda e, t=t, k=k: e.tensor_copy(out=HT[:, k, :], in_=t), reads=[tk], writes=["HT%d" % k])
                    h32_cb(k, t, tk)

        HTK = ["HT%d" % k for k in range(8)]

        def linear(lhs_list, rhs_list, blks, evac, reads):
            for (s, n) in blks:
                ps, pk = bank()
                nk = len(lhs_list)
                for i in range(nk):
                    P.pe(lambda e, ps=ps, i=i, s=s, n=n: e.matmul(ps[:lhs_list[i].shape[-1], 0:n], lhsT=lhs_list[i],
                                                                  rhs=rhs_list[i](s, n), start=(i == 0), stop=(i == nk - 1)),
                         reads=reads, writes=[pk])
                evac(ps, pk, s, n)

        ALLB = blocks_of(0, 2048, 512) + [(2048, 256)]

        def even_mixer(l):
            p = "l%d_" % l
            P.barrier()
            off = 0
            RS, off = carve(off, [NT], F32)
            T0, off = carve(off, [NT], F32)
            T1, off = carve(off, [NT], F32)
            norm_mod(0, 0, NT * 4)
            T2 = RS
            wb, off = carve(off, [8, 384], BF16)
            wkr, off = carve(off, [8, 2, 96], BF16)
            att_end = off
            ROPE, off = carve(off, [2, NT], F32)
            SQ, _ = carve(off, [NT], F32)
            convy, off = carve(off, [4, NT], BF16)
            cqn, off = carve(off, [2, NT], BF16)
            ckvn, off = carve(off, [NT], BF16)
            krr, off = carve(off, [NT], BF16)
            cw = smallp[:, 0:12].rearrange("p (a b) -> p a b", b=3)
            qng = smallp[:, 12:14]
            kvng = smallp[:, 14:15]
            P.dma("sp", "sm0", lambda e: e.dma_start(out=cw, in_=D[p + "convw"][:, :, :]), writes=["cw"])
            P.dma("sp", "sm1", lambda e: e.dma_start(out=smallp[:, 12:14], in_=D[p + "qng"][:, :]), writes=["qng"])
            P.dma("sp", "sm2", lambda e: e.dma_start(out=smallp[:, 14:15], in_=D[p + "kvng"][:, :]), writes=["kvng"])
            P.dma("sp", "rope", lambda e: e.dma_start(out=ROPE[0:96], in_=D["rope"][:, :, :]), writes=["ROPE"])
            P.dma("pool", "wkr", lambda e: e.dma_start(out=wkr, in_=D[p + "wkr"].rearrange("(k p) a b -> p k a b", p=128)),
                  writes=["wkr"])
            winv = D[p + "w_in"].rearrange("(k p) n -> p k n", p=128)
            wk = "wb"

            def hrhs(k):
                return lambda s, n: HT[:, k, s:s + n]

            def ev_copy(dst, dk):
                def f(ps, pk, s, n):
                    P.act(lambda e: e.activation(out=dst[:, s:s + n], in_=ps[:, 0:n], func=AF.Identity), reads=[pk], writes=[dk])
                return f

            P.dma("pool", "wb", lambda e: e.dma_start(out=wb[:, :, 0:384], in_=winv[:, :, 1536:1920]), writes=[wk])
            rd = HTK + [wk]

            def feat_norm(chunks, nfeat, gain, dst_fn, dkey):
                raws = [T0, T1]
                for i, co in enumerate(chunks):
                    linear([wb[:, k, co:co + 128] for k in range(8)], [hrhs(k) for k in range(8)], ALLB, ev_copy(raws[i], "T%d" % i), rd)
                bks = [bank() for _ in ALLB]
                for i in range(len(chunks)):
                    P.act(lambda e, i=i: e.activation(out=SQ, in_=raws[i], func=AF.Square), reads=["T%d" % i], writes=["SQ"])
                    for (ps, pk), (s, n) in zip(bks, ALLB):
                        P.pe(lambda e, ps=ps, s=s, n=n, i=i: e.matmul(ps[:, 0:n], lhsT=onesf[:], rhs=SQ[:, s:s + n],
                                                                   start=(i == 0), stop=(i == len(chunks) - 1)),
                             reads=["SQ", "onesf"], writes=[pk])
                for (ps, pk), (s, n) in zip(bks, ALLB):
                    P.act(lambda e, ps=ps, s=s, n=n: e.activation(out=RS[:, s:s + n], in_=ps[:, 0:n], func=AF.Sqrt,
                                                                  scale=1.0 / nfeat, bias=epsb[:, 0:1]), reads=[pk, "epsb"], writes=["RS"])
                P.dve(lambda e: e.reciprocal(out=RS, in_=RS), reads=["RS"], writes=["RS"])
                for i in range(len(chunks)):
                    P.dve(lambda e, i=i: e.scalar_tensor_tensor(out=dst_fn(i), in0=raws[i], scalar=gain[:, i:i + 1], in1=RS,
                                                             op0=ALU.mult, op1=ALU.mult),
                          reads=["T%d" % i, "RS", "qng", "kvng"], writes=[dkey])
            feat_norm([0, 128], 256.0, qng, lambda i: cqn[:, i, :], "cqn")
            feat_norm([256], 128.0, kvng, lambda i: ckvn, "ckvn")
            def ev_kr0(ps, pk, s, n):
                P.dve(lambda e: e.tensor_tensor(out=T0[0:96, s:s + n], in0=ps[0:96, 0:n], in1=ROPE[0:96, 0, s:s + n], op=ALU.mult),
                      reads=[pk, "ROPE"], writes=["T0"])
            linear([wkr[:, k, 0, :] for k in range(8)], [hrhs(k) for k in range(8)], ALLB, ev_kr0, HTK + ["wkr"])
            def ev_kr1(ps, pk, s, n):
                P.dve(lambda e: e.tensor_tensor(out=T1[0:96, s:s + n], in0=ps[0:96, 0:n], in1=ROPE[0:96, 1, s:s + n], op=ALU.mult),
                      reads=[pk, "ROPE"], writes=["T1"])
                P.dve(lambda e: e.tensor_tensor(out=krr[0:96, s:s + n], in0=T0[0:96, s:s + n], in1=T1[0:96, s:s + n], op=ALU.add),
                      reads=["T0", "T1"], writes=["krr"])
            linear([wkr[:, k, 1, :] for k in range(8)], [hrhs(k) for k in range(8)], ALLB, ev_kr1, HTK + ["wkr"])
            for c in range(4):
                for j in range(3):
                    P.dma("pool", "wb", lambda e, j=j, c=c: e.dma_start(
                        out=wb[:, :, j * 128:(j + 1) * 128], in_=winv[:, :, j * 512 + c * 128:j * 512 + (c + 1) * 128]),
                        writes=[wk])
                linear([wb[:, k, 128:256] for k in range(8)], [hrhs(k) for k in range(8)], ALLB, ev_copy(T0, "T0"), HTK + [wk])
                def ev_mul(ps, pk, s, n):
                    P.dve(lambda e: e.tensor_tensor(out=T0[:, s:s + n], in0=ps[:, 0:n], in1=T0[:, s:s + n], op=ALU.mult),
                          reads=[pk, "T0"], writes=["T0"])
                linear([wb[:, k, 256:384] for k in range(8)], [hrhs(k) for k in range(8)], ALLB, ev_mul, HTK + [wk])
                linear([wb[:, k, 0:128] for k in range(8)], [hrhs(k) for k in range(8)], ALLB, ev_copy(T1, "T1"), HTK + [wk])
                P.dve(lambda e, c=c: e.tensor_scalar(out=T2, in0=T0, scalar1=cw[:, c, 1:2], scalar2=None, op0=ALU.mult),
                      reads=["T0", "cw"], writes=["T2"])
                for (s, n) in (CTX, XS):
                    P.dve(lambda e, c=c, s=s, n=n: e.scalar_tensor_tensor(out=T2[:, s + 1:s + n], in0=T0[:, s:s + n - 1], scalar=cw[:, c, 0:1],
                                                                       in1=T2[:, s + 1:s + n], op0=ALU.mult, op1=ALU.add),
                          reads=["T0", "T2", "cw"], writes=["T2"])
                    P.dve(lambda e, c=c, s=s, n=n: e.scalar_tensor_tensor(out=T2[:, s:s + n - 1], in0=T0[:, s + 1:s + n], scalar=cw[:, c, 2:3],
                                                                       in1=T2[:, s:s + n - 1], op0=ALU.mult, op1=ALU.add),
                          reads=["T0", "T2", "cw"], writes=["T2"])
                P.dve(lambda e, c=c: e.tensor_tensor(out=convy[:, c, :], in0=T2, in1=T1, op=ALU.mult),
                      reads=["T2", "T1", "SQ"], writes=["convy", "SQ"])

            P.barrier()
            a0 = 0
            wuq, a0 = carve(a0, [2, 2, 768], BF16)
            wuk, a0 = carve(a0, [8, 96], BF16)
            wuv, a0 = carve(a0, [512], BF16)
            Kh = []; Vh = []; Qh = []; PT = []
            for i in range(2):
                t, a0 = carve(a0, [NT], BF16); Kh.append(t)
            for i in range(2):
                t, a0 = carve(a0, [18, 64], BF16); Vh.append(t)
            for i in range(2):
                t, a0 = carve(a0, [512], BF16); Qh.append(t)
            for i in range(4):
                t, a0 = carve(a0, [512], BF16); PT.append(t)
            RC, a0 = carve(a0, [512], F32)
            QT, a0 = carve(a0, [512], F32)
            assert a0 <= att_end, (a0, att_end)
            P.dma("pool", "wuq", lambda e: e.dma_start(out=wuq, in_=D[p + "w_uq"].rearrange("(k p) a n -> p k a n", p=128)), writes=["wuq"])
            P.dma("pool", "wuk", lambda e: e.dma_start(out=wuk, in_=D[p + "w_uk"][:, :, :]), writes=["wuk"])
            P.dma("pool", "wuv", lambda e: e.dma_start(out=wuv, in_=D[p + "w_uv"][:, :]), writes=["wuv"])
            scale = 96.0 ** -0.5
            pti = [0]
            for h in range(8):
                K = Kh[h % 2]; kk = "K%d" % (h % 2)
                V = Vh[h % 2]; vk = "V%d" % (h % 2)
                def ev_k(ps, pk, s, n, K=K, kk=kk):
                    P.dve(lambda e: e.tensor_tensor(out=K[0:96, s:s + n], in0=ps[0:96, 0:n], in1=krr[0:96, s:s + n], op=ALU.add),
                          reads=[pk, "krr"], writes=[kk])
                linear([wuk[:, h, :]], [lambda s, n: ckvn[:, s:s + n]], ALLB, ev_k, ["wuk", "ckvn"])
                for g in range(3):
                    ps, pk = bank()
                    for t6 in range(6):
                        ti = g * 6 + t6
                        P.pe(lambda e, ps=ps, t6=t6, ti=ti, h=h: e.matmul(ps[:, t6 * 64:(t6 + 1) * 64], lhsT=ckvn[:, ti * 128:(ti + 1) * 128],
                                                                        rhs=wuv[:, h * 64:(h + 1) * 64], start=True, stop=True),
                             reads=["ckvn", "wuv"], writes=[pk])
                    P.act(lambda e, ps=ps, g=g, V=V: e.activation(out=V[:, g * 6:(g + 1) * 6, :],
                                                                 in_=ps[:, 0:384].rearrange("p (a b) -> p a b", b=64), func=AF.Identity),
                          reads=[pk], writes=[vk])
                qblocks = [(0, 256, 2)] + [(256 + i * 512, 512, 18) for i in range(4)]
                for qi, (qs, qn, nkt) in enumerate(qblocks):
                    Q = Qh[qi % 2]; qk = "Q%d" % (qi % 2)
                    ps0, pk0 = bank()
                    ps1, pk1 = bank()
                    for kc in range(2):
                        P.pe(lambda e, kc=kc, ps0=ps0: e.matmul(ps0[0:96, 0:qn], lhsT=wuq[:, kc, 0, h * 96:(h + 1) * 96], rhs=cqn[:, kc, qs:qs + qn],
                                                               start=(kc == 0), stop=(kc == 1)), reads=["wuq", "cqn"], writes=[pk0])
                    for kc in range(2):
                        P.pe(lambda e, kc=kc, ps1=ps1: e.matmul(ps1[0:96, 0:qn], lhsT=wuq[:, kc, 1, h * 96:(h + 1) * 96], rhs=cqn[:, kc, qs:qs + qn],
                                                               start=(kc == 0), stop=(kc == 1)), reads=["wuq", "cqn"], writes=[pk1])
                    P.dve(lambda e, ps0=ps0: e.tensor_tensor(out=QT[0:96, 0:qn], in0=ps0[0:96, 0:qn], in1=ROPE[0:96, 0, qs:qs + qn], op=ALU.mult),
                          reads=[pk0, "ROPE"], writes=["QT"])
                    P.dve(lambda e, ps1=ps1: e.tensor_tensor(out=RC[0:96, 0:qn], in0=ps1[0:96, 0:qn], in1=ROPE[0:96, 1, qs:qs + qn], op=ALU.mult),
                          reads=[pk1, "ROPE"], writes=["RC"])
                    P.dve(lambda e, Q=Q: e.tensor_tensor(out=Q[0:96, 0:qn], in0=QT[0:96, 0:qn], in1=RC[0:96, 0:qn], op=ALU.add),
                          reads=["QT", "RC"], writes=[qk])
                    pnum, pnk = PS[6], "ps6"
                    pden, pdk = PS[7], "ps7"
                    for kt in range(nkt):
                        pss, psk = bank()
                        P.pe(lambda e, pss=pss, kt=kt, K=K, Q=Q: e.matmul(pss[:, 0:qn], lhsT=K[0:96, kt * 128:(kt + 1) * 128], rhs=Q[0:96, 0:qn],
                                                                        start=True, stop=True), reads=[kk, qk], writes=[psk])
                        pt = PT[pti[0] % 4]; ptk = "PT%d" % (pti[0] % 4); pti[0] += 1
                        P.act(lambda e, pss=pss, pt=pt: e.activation(out=pt[:, 0:qn], in_=pss[:, 0:qn], func=AF.Exp, scale=scale),
                              reads=[psk], writes=[ptk])
                        P.pe(lambda e, pt=pt, kt=kt, V=V: e.matmul(pnum[0:64, 0:qn], lhsT=V[:, kt, :], rhs=pt[:, 0:qn],
                                                                  start=(kt == 0), stop=(kt == nkt - 1)), reads=[vk, ptk], writes=[pnk])
                        P.pe(lambda e, pt=pt, kt=kt: e.matmul(pden[0:64, 0:qn], lhsT=onesb[:, 0:64], rhs=pt[:, 0:qn],
                                                              start=(kt == 0), stop=(kt == nkt - 1)), reads=["onesb", ptk], writes=[pdk])
                    P.dve(lambda e: e.reciprocal(out=RC[0:64, 0:qn], in_=pden[0:64, 0:qn]), reads=[pdk], writes=["RC"])
                    P.dve(lambda e: e.tensor_tensor(out=HT[0:64, h, qs:qs + qn], in0=pnum[0:64, 0:qn], in1=RC[0:64, 0:qn], op=ALU.mult),
                          reads=[pnk, "RC"], writes=["HT%d" % h])
            P.barrier()
            a0 = 0
            wob = []
            for i in range(2):
                t, a0 = carve(a0, [12, 128], BF16); wob.append(t)
            wov = D[p + "w_out"]
            for m in range(8):
                wo = wob[m % 2]; wok = "wo%d" % (m % 2)
                P.dma("pool", wok + "a", lambda e, wo=wo, m=m: e.dma_start(
                    out=wo[:, 0:4, :], in_=wov[0:512, m * 128:(m + 1) * 128].rearrange("(k p) n -> p k n", p=128)), writes=[wok + "a"])
                P.dma("pool", wok + "b", lambda e, wo=wo, m=m: e.dma_start(
                    out=wo[0:64, 4:12, :], in_=wov[512:1024, m * 128:(m + 1) * 128].rearrange("(k p) n -> p k n", p=64)), writes=[wok + "b"])
                lhs = [wo[:, c, :] for c in range(4)] + [wo[0:64, 4 + h, :] for h in range(8)]
                rhs = [(lambda c: lambda s, n: convy[:, c, s:s + n])(c) for c in range(4)] + \
                      [(lambda h: lambda s, n: HT[0:64, h, s:s + n])(h) for h in range(8)]
                for col, seg in ((1, [(0, 256)]), (0, blocks_of(256, 2048, 512))):
                    def ev(ps, pk, s, n, m=m, col=col):
                        P.dve(lambda e: e.scalar_tensor_tensor(out=XT[:, m, s:s + n], in0=ps[:, 0:n], scalar=gate_ap(0, m, col),
                                                               in1=XT[:, m, s:s + n], op0=ALU.mult, op1=ALU.add),
                              reads=[pk, "XT", "macc"], writes=["XT"])
                    linear(lhs, rhs, seg, ev, ["convy", wok + "a", wok + "b"] + HTK)

        def odd_mixer(l):
            p = "l%d_" % l
            P.barrier()
            off = 0
            RS, off = carve(off, [NT], F32)
            T0, off = carve(off, [NT], F32)
            T1, off = carve(off, [NT], F32)
            norm_mod(0, 0, NT * 4)
            tmp_end = off
            masks, off = carve(off, [6, 128], F32)
            ABr, off = carve(off, [18, 32], F32)
            Gt, off = carve(off, [18, 16], F32)
            BE, off = carve(off, [18, 16], F32)
            NBE, off = carve(off, [18, 16], F32)
            GC, off = carve(off, [18, 32], F32)
            EG, off = carve(off, [18, 16], F32)
            NEG, off = carve(off, [18, 16], F32)
            EKD, off = carve(off, [18, 16], F32)
            GEND, off = carve(off, [18, 16], F32)
            alog, off = carve(off, [16], F32)
            dtb, off = carve(off, [16], F32)
            ong, off = carve(off, [128], F32)
            cwo, off = carve(off, [24, 3], F32)
            one1, off = carve(off, [1], F32)
            S_, off = carve(off, [128], F32)
            Sbf, off = carve(off, [128], BF16)
            wab, off = carve(off, [8, 32], BF16)
            qT, off = carve(off, [NT], BF16)
            kT, off = carve(off, [NT], BF16)
            vT = None
            ktm, off = carve(off, [18, 128], BF16)
            vtm, off = carve(off, [18, 128], BF16)
            Zst, off = carve(off, [18, 128], BF16)
            vTa, _ = carve(off, [NT], BF16)
            AT, off = carve(off, [18, 128], BF16)
            Oacc, off = carve(off, [18, 128], F32)
            whq, off = carve(off, [8, 512], BF16)
            woh, off = carve(off, [1024], BF16)
            t0 = 0
            Gr, t0 = carve(t0, [4, 128], F32)
            E_, t0 = carve(t0, [4, 128], F32)
            Em, t0 = carve(t0, [4, 128], F32)
            EmS, t0 = carve(t0, [4, 128], F32)
            Pb = []; Qb = []
            for i in range(2):
                t, t0 = carve(t0, [4, 128], F32); Pb.append(t)
            for i in range(2):
                t, t0 = carve(t0, [4, 128], F32); Qb.append(t)
            Zw, t0 = carve(t0, [4, 128], F32)
            rr, t0 = carve(t0, [128], BF16)
            vnew, t0 = carve(t0, [128], BF16)
            kd, t0 = carve(t0, [128], BF16)
            tmpo, t0 = carve(t0, [128], F32)
            tmp2, t0 = carve(t0, [128], F32)
            c0 = 0
            SQc, c0 = carve(c0, [18, 128], F32)
            ssb, c0 = carve(c0, [18], F32)
            ytm, c0 = carve(c0, [18, 128], BF16)
            yT, c0 = carve(c0, [NT], BF16)
            assert c0 <= tmp_end, (c0, tmp_end)
            winv = D[p + "w_in"].rearrange("(k p) n -> p k n", p=128)
            P.dma("sp", "o0", lambda e: e.dma_start(out=masks, in_=D["masks"][:, :, :]), writes=["masks"])
            P.dma("sp", "o1", lambda e: e.dma_start(out=alog, in_=D[p + "alog"][:, :]), writes=["alog"])
            P.dma("sp", "o2", lambda e: e.dma_start(out=dtb, in_=D[p + "dtb"][:, :]), writes=["dtb"])
            P.dma("sp", "o3", lambda e: e.dma_start(out=ong, in_=D[p + "ong"][:, :]), writes=["ong"])
            P.dma("sp", "o4", lambda e: e.dma_start(out=cwo, in_=D[p + "convw"][:, :, :]), writes=["cwo"])
            P.dma("pool", "wab", lambda e: e.dma_start(out=wab, in_=winv[:, :, 4096:4128]), writes=["wab"])
            P.dve(lambda e: e.memset(one1, 1.0), writes=["one1"])
            for g0 in range(0, 18, 16):
                ps, pk = bank()
                tl = list(range(g0, min(18, g0 + 16)))
                for ti in tl:
                    for k in range(8):
                        P.pe(lambda e: e.matmul(ps[:, (ti - g0) * 32:(ti - g0 + 1) * 32], lhsT=HT[:, k, ti * 128:(ti + 1) * 128], rhs=wab[:, k, :],
                                                start=(k == 0), stop=(k == 7)), reads=["HT%d" % k, "wab"], writes=[pk])
                P.act(lambda e: e.activation(out=ABr[:, g0:g0 + len(tl), :], in_=ps[:, 0:32 * len(tl)].rearrange("p (a b) -> p a b", b=32), func=AF.Identity),
                      reads=[pk], writes=["ABr"])
            b16 = lambda a: a[:, None, :].broadcast_to([128, 18, 16])
            P.dve(lambda e: e.tensor_tensor(out=Gt, in0=ABr[:, :, 0:16], in1=b16(dtb), op=ALU.add), reads=["ABr", "dtb"], writes=["Gt"])
            P.act(lambda e: e.activation(out=Gt, in_=Gt, func=AF.Exp), reads=["Gt"], writes=["Gt"])
            P.act(lambda e: e.activation(out=Gt, in_=Gt, func=AF.Ln, bias=one1[:, 0:1], scale=1.0), reads=["Gt", "one1"], writes=["Gt"])
            P.act(lambda e: e.activation(out=alog, in_=alog, func=AF.Exp), reads=["alog"], writes=["alog"])
            P.dve(lambda e: e.scalar_tensor_tensor(out=Gt, in0=Gt, scalar=-1.0, in1=b16(alog), op0=ALU.mult, op1=ALU.mult), reads=["Gt", "alog"], writes=["Gt"])
            P.act(lambda e: e.activation(out=BE, in_=ABr[:, :, 16:32], func=AF.Sigmoid), reads=["ABr"], writes=["BE"])
            P.dve(lambda e: e.tensor_scalar(out=NBE, in0=BE, scalar1=-1.0, scalar2=None, op0=ALU.mult), reads=["BE"], writes=["NBE"])
            for g0 in range(0, 18, 16):
                ps, pk = bank()
                tl = list(range(g0, min(18, g0 + 16)))
                for ti in tl:
                    c = (ti - g0) * 32
                    P.pe(lambda e: e.matmul(ps[:, c:c + 8], lhsT=masks[:, 0, :], rhs=Gt[:, ti, 0:8], start=True, stop=True), reads=["masks", "Gt"], writes=[pk])
                    P.pe(lambda e: e.matmul(ps[:, c + 8:c + 16], lhsT=masks[:, 1, :], rhs=Gt[:, ti, 8:16], start=True, stop=True), reads=["masks", "Gt"], writes=[pk])
                    P.pe(lambda e: e.matmul(ps[:, c + 16:c + 32], lhsT=onesf[:], rhs=Gt[:, ti, :], start=True, stop=True), reads=["onesf", "Gt"], writes=[pk])
                P.act(lambda e: e.activation(out=GC[:, g0:g0 + len(tl), :], in_=ps[:, 0:32 * len(tl)].rearrange("p (a b) -> p a b", b=32), func=AF.Identity),
                      reads=[pk], writes=["GC"])
            P.act(lambda e: e.activation(out=EG, in_=GC[:, :, 0:16], func=AF.Exp), reads=["GC"], writes=["EG"])
            P.dve(lambda e: e.tensor_scalar(out=NEG, in0=EG, scalar1=-1.0, scalar2=None, op0=ALU.mult), reads=["EG"], writes=["NEG"])
            P.dve(lambda e: e.tensor_tensor(out=EKD, in0=GC[:, :, 16:32], in1=GC[:, :, 0:16], op=ALU.subtract), reads=["GC"], writes=["EKD"])
            P.act(lambda e: e.activation(out=EKD, in_=EKD, func=AF.Exp), reads=["EKD"], writes=["EKD"])
            P.act(lambda e: e.activation(out=GEND, in_=GC[:, :, 16:32], func=AF.Exp), reads=["GC"], writes=["GEND"])
            if _DBG.get("stop_pre"):
                fl = lambda a: a.rearrange("p a b -> p (a b)")
                for src_, k_, o_ in ((Gt, 0, 0), (BE, 0, 288), (GC, 1, 0), (EG, 2, 0), (EKD, 2, 288), (GEND, 3, 0), (ABr, 4, 0)):
                    n_ = src_.shape[1] * src_.shape[2]
                    P.dve(lambda e: e.tensor_copy(out=XT[:, k_, o_:o_ + n_], in_=fl(src_)), reads=["Gt", "BE", "GC", "EG", "EKD", "GEND", "ABr"], writes=["XT"])
                return

            def hrhs(k):
                return lambda s, n: HT[:, k, s:s + n]
            tiles4 = [list(range(i, min(18, i + 4))) for i in range(0, 18, 4)]
            for h in range(8):
                P.barrier()
                for j in range(4):
                    P.dma("pool", "whq%d" % j, lambda e: e.dma_start(out=whq[:, :, j * 128:(j + 1) * 128],
                                                                   in_=winv[:, :, j * 1024 + h * 128:j * 1024 + (h + 1) * 128]), writes=["whq%d" % j])
                _step("whq dma")
                P.dma("pool", "woh", lambda e: e.dma_start(out=woh, in_=D[p + "w_out"][h * 128:(h + 1) * 128, :]), writes=["woh"])
                _step("woh dma")
                for j, dst, dk in ((0, qT, "qT"), (1, kT, "kT"), (2, vTa, "vTa")):
                    ci = j * 8 + h
                    def ev(ps, pk, s, n):
                        P.act(lambda e: e.activation(out=T0[:, s:s + n], in_=ps[:, 0:n], func=AF.Identity), reads=[pk], writes=["T0"])
                    linear([whq[:, k, j * 128:(j + 1) * 128] for k in range(8)], [hrhs(k) for k in range(8)], ALLB, ev, HTK + ["whq%d" % j])
                    _step("proj %d" % j)
                    P.dve(lambda e: e.tensor_scalar(out=T1, in0=T0, scalar1=cwo[:, ci, 1:2], scalar2=None, op0=ALU.mult), reads=["T0", "cwo"], writes=["T1"])
                    for (s, n) in (CTX, XS):
                        P.dve(lambda e: e.scalar_tensor_tensor(out=T1[:, s + 1:s + n], in0=T0[:, s:s + n - 1], scalar=cwo[:, ci, 0:1],
                                                               in1=T1[:, s + 1:s + n], op0=ALU.mult, op1=ALU.add), reads=["T0", "T1", "cwo"], writes=["T1"])
                        P.dve(lambda e: e.scalar_tensor_tensor(out=T1[:, s:s + n - 1], in0=T0[:, s + 1:s + n], scalar=cwo[:, ci, 2:3],
                                                               in1=T1[:, s:s + n - 1], op0=ALU.mult, op1=ALU.add), reads=["T0", "T1", "cwo"], writes=["T1"])
                    _step("conv %d" % j)
                    if j == 2:
                        P.act(lambda e: e.activation(out=dst, in_=T1, func=AF.Silu), reads=["T1"], writes=[dk])
                    else:
                        P.act(lambda e: e.activation(out=T1, in_=T1, func=AF.Silu), reads=["T1"], writes=["T1"])
                        P.act(lambda e: e.activation(out=T0, in_=T1, func=AF.Square), reads=["T1", "T0"], writes=["T0"])
                        for (s, n) in ALLB:
                            ps, pk = bank()
                            P.pe(lambda e: e.matmul(ps[:, 0:n], lhsT=onesf[:], rhs=T0[:, s:s + n], start=True, stop=True), reads=["T0", "onesf"], writes=[pk])
                            P.act(lambda e: e.activation(out=RS[:, s:s + n], in_=ps[:, 0:n], func=AF.Sqrt, scale=1.0, bias=epsb[:, 0:1]), reads=[pk, "epsb"], writes=["RS"])
                        P.dve(lambda e: e.reciprocal(out=RS, in_=RS), reads=["RS"], writes=["RS"])
                        sc_ = (128.0 ** -0.5) if j == 0 else 1.0
                        P.dve(lambda e: e.scalar_tensor_tensor(out=dst, in0=T1, scalar=sc_, in1=RS, op0=ALU.mult, op1=ALU.mult), reads=["T1", "RS"], writes=[dk])
                    _step("l2norm %d" % j)
                _step("A proj done")
                for src, sk, dstm, dmk in ((kT, "kT", ktm, "ktm"), (vTa, "vTa", vtm, "vtm")):
                    for tl in tiles4:
                        ps, pk = bank()
                        for jj, ti in enumerate(tl):
                            P.pe(lambda e: e.matmul(ps[:, jj * 128:(jj + 1) * 128], lhsT=src[:, ti * 128:(ti + 1) * 128], rhs=identb[:], start=True, stop=True),
                                 reads=[sk, "identb"], writes=[pk])
                        P.act(lambda e: e.activation(out=dstm[:, tl[0]:tl[0] + len(tl), :], in_=ps[:, 0:128 * len(tl)].rearrange("p (a b) -> p a b", b=128), func=AF.Identity),
                              reads=[pk], writes=[dmk])
                    _step("transposes " + dmk)
                if _DBG.get("stop_A") and h == _DBG.get("dbg_head", 0):
                    P.dve(lambda e: e.tensor_copy(out=XT[:, 0, :], in_=qT), reads=["qT"], writes=["XT"])
                    P.dve(lambda e: e.tensor_copy(out=XT[:, 1, :], in_=kT), reads=["kT"], writes=["XT"])
                    P.dve(lambda e: e.tensor_copy(out=XT[:, 2, :], in_=vtm.rearrange("p a b -> p (a b)")), reads=["vtm"], writes=["XT"])
                    P.dve(lambda e: e.tensor_copy(out=XT[:, 3, :], in_=ktm.rearrange("p a b -> p (a b)")), reads=["ktm"], writes=["XT"])
                    return
                P.barrier()
                for dr in range(2):
                    col = dr * 8 + h
                    mg, mdt, mincl, mstr = (0, 2, 0, 3) if dr == 0 else (1, 3, 1, 2)
                    for tl in tiles4:
                        nt4 = len(tl); t0_ = tl[0]; W = nt4 * 128
                        bcm = lambda mi: masks[:, mi, :][:, None, :].broadcast_to([128, nt4, 128])
                        gsl = Gt[:, t0_:t0_ + nt4, col:col + 1].broadcast_to([128, nt4, 128])
                        P.dve(lambda e: e.tensor_tensor(out=Gr[:, 0:nt4, :], in0=bcm(mg), in1=gsl, op=ALU.mult), reads=["masks", "Gt"], writes=["Gr"])
                        ps, pk = bank()
                        P.pe(lambda e: e.matmul(ps[:, 0:W], lhsT=masks[:, mdt, :], rhs=Gr[:, 0:nt4, :].rearrange("p a b -> p (a b)"), start=True, stop=True),
                             reads=["masks", "Gr"], writes=[pk])
                        P.act(lambda e: e.activation(out=E_[:, 0:nt4, :], in_=ps[:, 0:W].rearrange("p (a b) -> p a b", b=128), func=AF.Exp), reads=[pk], writes=["E_"])
                        P.dve(lambda e: e.tensor_tensor(out=Em[:, 0:nt4, :], in0=E_[:, 0:nt4, :], in1=bcm(mincl), op=ALU.mult), reads=["E_", "masks"], writes=["Em"])
                        P.dve(lambda e: e.tensor_tensor(out=EmS[:, 0:nt4, :], in0=E_[:, 0:nt4, :], in1=bcm(mstr), op=ALU.mult), reads=["E_", "masks"], writes=["EmS"])
                        nb_ = NBE[:, t0_:t0_ + nt4, col:col + 1].broadcast_to([128, nt4, 128])
                        P.dve(lambda e: e.tensor_tensor(out=EmS[:, 0:nt4, :], in0=EmS[:, 0:nt4, :], in1=nb_, op=ALU.mult), reads=["EmS", "NBE"], writes=["EmS"])
                        pkk, pkkk = bank()
                        pkq, pkqk = bank()
                        for jj, ti in enumerate(tl):
                            P.pe(lambda e: e.matmul(pkk[:, jj * 128:(jj + 1) * 128], lhsT=kT[:, ti * 128:(ti + 1) * 128], rhs=kT[:, ti * 128:(ti + 1) * 128], start=True, stop=True),
                                 reads=["kT"], writes=[pkkk])
                            P.pe(lambda e: e.matmul(pkq[:, jj * 128:(jj + 1) * 128], lhsT=kT[:, ti * 128:(ti + 1) * 128], rhs=qT[:, ti * 128:(ti + 1) * 128], start=True, stop=True),
                                 reads=["kT", "qT"], writes=[pkqk])
                        r3 = lambda a: a[:, 0:W].rearrange("p (a b) -> p a b", b=128)
                        P.dve(lambda e: e.tensor_tensor(out=AT[:, t0_:t0_ + nt4, :], in0=r3(pkq), in1=Em[:, 0:nt4, :], op=ALU.mult), reads=[pkqk, "Em"], writes=["AT"])
                        halves = [(0, min(2, nt4))] + ([(2, nt4)] if nt4 > 2 else [])
                        hk = lambda base, hi: "%sh%d" % (base, hi)
                        allh = lambda base: [hk(base, hi) for hi in range(len(halves))]
                        r3h = lambda a_, lo, hi_: a_[:, lo * 128:hi_ * 128].rearrange("p (a b) -> p a b", b=128)
                        Pc, Qc = Pb[0], Qb[0]
                        P.dve(lambda e: e.tensor_tensor(out=Pc[:, 0:nt4, :], in0=r3(pkk), in1=EmS[:, 0:nt4, :], op=ALU.mult), reads=[pkkk, "EmS"], writes=allh("Pb0"))
                        for hi, (lo, up) in enumerate(halves):
                            ps, pk = bank()
                            for jj in range(lo, up):
                                P.pe(lambda e: e.matmul(ps[:, jj * 128:(jj + 1) * 128], lhsT=Pc[:, jj, :], rhs=identf[:], start=True, stop=True), reads=[hk("Pb0", hi), "identf"], writes=[pk])
                            P.act(lambda e: e.activation(out=Qc[:, lo:up, :], in_=r3h(ps, lo, up), func=AF.Identity), reads=[pk], writes=[hk("Qb0", hi)])
                            P.dve(lambda e: e.tensor_tensor(out=Zw[:, lo:up, :], in0=Pc[:, lo:up, :], in1=identf[:][:, None, :].broadcast_to([128, up - lo, 128]), op=ALU.add),
                                  reads=[hk("Pb0", hi), "identf"], writes=[hk("Zw", hi)])
                        cur = 0
                        for it in range(1, 7):
                            nx = 1 - cur
                            Pc, Qc, Pn, Qn = Pb[cur], Qb[cur], Pb[nx], Qb[nx]
                            pck, qck, pnk_, qnk = "Pb%d" % cur, "Qb%d" % cur, "Pb%d" % nx, "Qb%d" % nx
                            for hi, (lo, up) in enumerate(halves):
                                ps, pk = bank()
                                for jj in range(lo, up):
                                    P.pe(lambda e: e.matmul(ps[:, jj * 128:(jj + 1) * 128], lhsT=Pc[:, jj, :], rhs=Qc[:, jj, :], start=True, stop=True), reads=[hk(pck, hi), hk(qck, hi)], writes=[pk])
                                P.act(lambda e: e.activation(out=Qn[:, lo:up, :], in_=r3h(ps, lo, up), func=AF.Identity), reads=[pk], writes=[hk(qnk, hi)])
                            if it < 6:
                                for hi, (lo, up) in enumerate(halves):
                                    ps2, pk2 = bank()
                                    for jj in range(lo, up):
                                        P.pe(lambda e: e.matmul(ps2[:, jj * 128:(jj + 1) * 128], lhsT=Qc[:, jj, :], rhs=Pc[:, jj, :], start=True, stop=True), reads=[hk(pck, hi), hk(qck, hi)], writes=[pk2])
                                    P.act(lambda e: e.activation(out=Pn[:, lo:up, :], in_=r3h(ps2, lo, up), func=AF.Identity), reads=[pk2], writes=[hk(pnk_, hi)])
                            for hi, (lo, up) in enumerate(halves):
                                ps3, pk3 = bank()
                                for jj in range(lo, up):
                                    P.pe(lambda e: e.matmul(ps3[:, jj * 128:(jj + 1) * 128], lhsT=Qn[:, jj, :], rhs=Zw[:, jj, :], start=True, stop=True), reads=[hk(qnk, hi), hk("Zw", hi)], writes=[pk3])
                                dstz = Zst[:, t0_ + lo:t0_ + up, :] if it == 6 else Zw[:, lo:up, :]
                                P.dve(lambda e: e.tensor_tensor(out=dstz, in0=r3h(ps3, lo, up), in1=Zw[:, lo:up, :], op=ALU.add), reads=[pk3, hk("Zw", hi)],
                                      writes=["Zst", hk("Zw", hi)] if it == 6 else [hk("Zw", hi)])
                            cur = nx
                    if _DBG.get("stop_chain"):
                        return
                    order = list(range(18)) if dr == 0 else [1, 0] + list(range(17, 1, -1))
                    P.dve(lambda e: e.memset(S_, 0.0), writes=["S"])
                    P.dve(lambda e: e.memset(Sbf, 0.0), writes=["Sbf"])
                    for ti in order:
                        tsl = slice(ti * 128, (ti + 1) * 128)
                        psA, pka = bank()
                        P.pe(lambda e: e.matmul(psA[:, 0:128], lhsT=kT[:, tsl], rhs=Sbf, start=True, stop=True), reads=["kT", "Sbf"], writes=[pka])
                        P.pe(lambda e: e.matmul(psA[:, 128:256], lhsT=qT[:, tsl], rhs=Sbf, start=True, stop=True), reads=["qT", "Sbf"], writes=[pka])
                        P.dve(lambda e: e.scalar_tensor_tensor(out=rr, in0=psA[:, 0:128], scalar=NEG[:, ti, col:col + 1], in1=vtm[:, ti, :], op0=ALU.mult, op1=ALU.add),
                              reads=[pka, "NEG", "vtm"], writes=["rr"])
                        P.dve(lambda e: e.tensor_scalar(out=tmpo, in0=psA[:, 128:256], scalar1=EG[:, ti, col:col + 1], scalar2=None, op0=ALU.mult), reads=[pka, "EG"], writes=["tmpo"])
                        psB, pkb = bank()
                        P.pe(lambda e: e.matmul(psB[:, 0:128], lhsT=Zst[:, ti, :], rhs=rr, start=True, stop=True), reads=["Zst", "rr"], writes=[pkb])
                        P.dve(lambda e: e.tensor_scalar(out=vnew, in0=psB[:, 0:128], scalar1=BE[:, ti, col:col + 1], scalar2=None, op0=ALU.mult), reads=[pkb, "BE"], writes=["vnew"])
                        P.dve(lambda e: e.tensor_scalar(out=kd, in0=ktm[:, ti, :], scalar1=EKD[:, ti, col:col + 1], scalar2=None, op0=ALU.mult), reads=["ktm", "EKD"], writes=["kd"])
                        psC, pkc = bank()
                        P.pe(lambda e: e.matmul(psC[:, 0:128], lhsT=AT[:, ti, :], rhs=vnew, start=True, stop=True), reads=["AT", "vnew"], writes=[pkc])
                        P.pe(lambda e: e.matmul(psC[:, 128:256], lhsT=kd, rhs=vnew, start=True, stop=True), reads=["kd", "vnew"], writes=[pkc])
                        if dr == 0:
                            P.dve(lambda e: e.tensor_tensor(out=Oacc[:, ti, :], in0=psC[:, 0:128], in1=tmpo, op=ALU.add), reads=[pkc, "tmpo"], writes=["Oacc"])
                        else:
                            P.dve(lambda e: e.tensor_tensor(out=tmp2, in0=psC[:, 0:128], in1=tmpo, op=ALU.add), reads=[pkc, "tmpo"], writes=["tmp2"])
                            P.dve(lambda e: e.tensor_tensor(out=Oacc[:, ti, :], in0=Oacc[:, ti, :], in1=tmp2, op=ALU.add), reads=["tmp2", "Oacc"], writes=["Oacc"])
                        P.dve(lambda e: e.scalar_tensor_tensor(out=S_, in0=S_, scalar=GEND[:, ti, col:col + 1], in1=psC[:, 128:256], op0=ALU.mult, op1=ALU.add),
                              reads=[pkc, "S", "GEND"], writes=["S"])
                        P.act(lambda e: e.activation(out=Sbf, in_=S_, func=AF.Identity), reads=["S"], writes=["Sbf"])
                if _DBG.get("stop_B") and h == _DBG.get("dbg_head", 0):
                    P.dve(lambda e: e.tensor_copy(out=XT[:, 0, :], in_=Oacc.rearrange("p a b -> p (a b)")), reads=["Oacc"], writes=["XT"])
                    P.dve(lambda e: e.tensor_copy(out=XT[:, 1, :], in_=Zst.rearrange("p a b -> p (a b)")), reads=["Zst"], writes=["XT"])
                    P.dve(lambda e: e.tensor_copy(out=XT[:, 2, :], in_=AT.rearrange("p a b -> p (a b)")), reads=["AT"], writes=["XT"])
                    return
                P.barrier()
                P.dve(lambda e: e.tensor_tensor(out=SQc, in0=Oacc, in1=Oacc, op=ALU.mult), reads=["Oacc"], writes=["SQc"])
                P.dve(lambda e: e.tensor_reduce(out=ssb, in_=SQc, axis=AX.X, op=ALU.add), reads=["SQc"], writes=["ssb"])
                P.act(lambda e: e.activation(out=ssb, in_=ssb, func=AF.Sqrt, scale=1.0 / 128, bias=epsb[:, 0:1]), reads=["ssb", "epsb"], writes=["ssb"])
                P.dve(lambda e: e.reciprocal(out=ssb, in_=ssb), reads=["ssb"], writes=["ssb"])
                P.dve(lambda e: e.tensor_tensor(out=Oacc, in0=Oacc, in1=ssb[:, :, None].broadcast_to([128, 18, 128]), op=ALU.mult), reads=["Oacc", "ssb"], writes=["Oacc"])
                P.dve(lambda e: e.tensor_tensor(out=Oacc, in0=Oacc, in1=ong[:, None, :].broadcast_to([128, 18, 128]), op=ALU.mult), reads=["Oacc", "ong"], writes=["Oacc"])
                for tl in tiles4:
                    ps, pk = bank()
                    for jj, ti in enumerate(tl):
                        for k in range(8):
                            P.pe(lambda e: e.matmul(ps[:, jj * 128:(jj + 1) * 128], lhsT=HT[:, k, ti * 128:(ti + 1) * 128], rhs=whq[:, k, 384:512], start=(k == 0), stop=(k == 7)),
                                 reads=["HT%d" % k, "whq3"], writes=[pk])
                    P.act(lambda e: e.activation(out=SQc[:, tl[0]:tl[0] + len(tl), :], in_=ps[:, 0:128 * len(tl)].rearrange("p (a b) -> p a b", b=128), func=AF.Silu),
                          reads=[pk], writes=["SQc"])
                P.dve(lambda e: e.tensor_tensor(out=ytm, in0=Oacc, in1=SQc, op=ALU.mult), reads=["Oacc", "SQc"], writes=["ytm"])
                for tl in tiles4:
                    ps, pk = bank()
                    for jj, ti in enumerate(tl):
                        P.pe(lambda e: e.matmul(ps[:, jj * 128:(jj + 1) * 128], lhsT=ytm[:, ti, :], rhs=identb[:], start=True, stop=True), reads=["ytm", "identb"], writes=[pk])
                    P.act(lambda e: e.activation(out=yT[:, tl[0] * 128:(tl[0] + len(tl)) * 128], in_=ps[:, 0:128 * len(tl)], func=AF.Identity), reads=[pk], writes=["vT"])
                if _DBG.get("stop_C") and h == _DBG.get("dbg_head", 0):
                    P.dve(lambda e: e.tensor_copy(out=XT[:, 0, :], in_=ytm.rearrange("p a b -> p (a b)")), reads=["ytm"], writes=["XT"])
                    P.dve(lambda e: e.tensor_copy(out=XT[:, 1, :], in_=yT), reads=["vT"], writes=["XT"])
                    P.dve(lambda e: e.tensor_copy(out=XT[:, 2, :], in_=SQc.rearrange("p a b -> p (a b)")), reads=["SQc"], writes=["XT"])
                    P.dve(lambda e: e.tensor_copy(out=XT[:, 3, :], in_=Oacc.rearrange("p a b -> p (a b)")), reads=["Oacc"], writes=["XT"])
                    return
                for m in range(8):
                    for col_, seg in ((1, [(0, 256)]), (0, blocks_of(256, 2048, 512))):
                        def ev(ps, pk, s, n):
                            P.dve(lambda e: e.scalar_tensor_tensor(out=XT[:, m, s:s + n], in0=ps[:, 0:n], scalar=gate_ap(0, m, col_),
                                                                   in1=XT[:, m, s:s + n], op0=ALU.mult, op1=ALU.add), reads=[pk, "XT", "macc"], writes=["XT"])
                        linear([woh[:, m * 128:(m + 1) * 128]], [lambda s, n: yT[:, s:s + n]], seg, ev, ["woh", "vT"])

        def ffn(l, tok_groups, moe):
            p = "l%d_" % l
            P.barrier()
            off = 0
            RS, off = carve(off, [NT], F32)
            nf = 28 if moe else 22
            ne = 8 if moe else 1
            fdim = nf * 128
            if moe:
                GOFF = ARB - 6 * 1024
                LG, off2 = carve(GOFF, [18, 8], F32)
                rw, off2 = carve(off2, [8, 8], F32)
                P.dma("sp", "rw", lambda e: e.dma_start(out=rw, in_=D[p + "router"].rearrange("(k p) n -> p k n", p=128)), writes=["rw"])

                def h32_cb(k, t, tk):
                    ps, pk = bank()
                    for ti in range(18):
                        P.pe(lambda e, ps=ps, ti=ti, t=t, k=k: e.matmul(ps[:, ti * 8:(ti + 1) * 8], lhsT=t[:, ti * 128:(ti + 1) * 128], rhs=rw[:, k, :],
                                                                     start=True, stop=True), reads=[tk, "rw"], writes=[pk])
                    lgf = LG.rearrange("p a b -> p (a b)")
                    if k == 0:
                        P.dve(lambda e, ps=ps: e.tensor_copy(out=lgf, in_=ps[:, 0:144]), reads=[pk], writes=["LG"])
                    else:
                        P.dve(lambda e, ps=ps: e.tensor_tensor(out=lgf, in0=lgf, in1=ps[:, 0:144], op=ALU.add), reads=[pk, "LG"], writes=["LG"])
                norm_mod(1, 0, NT * 4, h32_cb)
                o3 = off2
                M1, o3 = carve(o3, [18], F32); M2, o3 = carve(o3, [18], F32)
                E1, o3 = carve(o3, [18, 8], F32); E2, o3 = carve(o3, [18, 8], F32); L2, o3 = carve(o3, [18, 8], F32)
                GT, o3 = carve(o3, [18, 8], F32)
                DG, o3 = carve(o3, [128], F32)
                bc = lambda a: a[:, :, None].broadcast_to([128, 18, 8])
                P.dve(lambda e: e.tensor_reduce(out=M1, in_=LG, axis=AX.X, op=ALU.max), reads=["LG"], writes=["M1"])
                P.dve(lambda e: e.tensor_tensor(out=E1, in0=LG, in1=bc(M1), op=ALU.is_equal), reads=["LG", "M1"], writes=["E1"])
                P.dve(lambda e: e.scalar_tensor_tensor(out=L2, in0=E1, scalar=-1e30, in1=LG, op0=ALU.mult, op1=ALU.add), reads=["E1", "LG"], writes=["L2"])
                P.dve(lambda e: e.tensor_reduce(out=M2, in_=L2, axis=AX.X, op=ALU.max), reads=["L2"], writes=["M2"])
                P.dve(lambda e: e.tensor_tensor(out=E2, in0=L2, in1=bc(M2), op=ALU.is_equal), reads=["L2", "M2"], writes=["E2"])
                P.dve(lambda e: e.tensor_tensor(out=M2, in0=M2, in1=M1, op=ALU.subtract), reads=["M1", "M2"], writes=["M2"])
                P.act(lambda e: e.activation(out=M2, in_=M2, func=AF.Exp), reads=["M2"], writes=["M2"])
                P.dve(lambda e: e.tensor_scalar(out=M1, in0=M2, scalar1=1.0, scalar2=None, op0=ALU.add), reads=["M2"], writes=["M1"])
                P.dve(lambda e: e.reciprocal(out=M1, in_=M1), reads=["M1"], writes=["M1"])
                P.dve(lambda e: e.tensor_tensor(out=M2, in0=M2, in1=M1, op=ALU.mult), reads=["M1", "M2"], writes=["M2"])
                P.dve(lambda e: e.tensor_tensor(out=E1, in0=E1, in1=bc(M1), op=ALU.mult), reads=["E1", "M1"], writes=["E1"])
                P.dve(lambda e: e.tensor_tensor(out=E2, in0=E2, in1=bc(M2), op=ALU.mult), reads=["E2", "M2"], writes=["E2"])
                P.dve(lambda e: e.tensor_tensor(out=GT, in0=E1, in1=E2, op=ALU.add), reads=["E1", "E2"], writes=["GT"])
                a0 = NT * 4
                alim = GOFF
            else:
                norm_mod(1, 0, NT * 4)
                a0 = NT * 4
                alim = ARB
            P.barrier()
            gmax = max(n for (_, n, _) in tok_groups)
            ACT_, a0 = carve(a0, [nf, gmax], BF16)
            GE, a0 = carve(a0, [gmax], F32)
            TS, a0 = carve(a0, [512], F32)
            TU, a0 = carve(a0, [512], F32)
            w1b = []; w2b = []
            for i in range(2):
                t, a0 = carve(a0, [8, 256], BF16); w1b.append(t)
            for i in range(2):
                t, a0 = carve(a0, [nf, 128], BF16); w2b.append(t)
            assert a0 <= alim, (a0, alim)
            cnt1 = [0]; cnt2 = [0]
            for (gs, gn, blks) in tok_groups:
                for ex in range(ne):
                    if moe:
                        w1v = D[p + "moe_w1"][ex].rearrange("(k p) n -> p k n", p=128)
                        w2v = D[p + "moe_w2"][ex]
                        for ti in range(gs // 128, (gs + gn) // 128):
                            P.dve(lambda e, ti=ti, ex=ex: e.tensor_scalar(out=DG, in0=identf[:], scalar1=GT[:, ti, ex:ex + 1], scalar2=None, op0=ALU.mult),
                                  reads=["GT", "identf"], writes=["DG"])
                            ps, pk = bank()
                            P.pe(lambda e, ps=ps: e.matmul(ps[:, 0:128], lhsT=onesf[:], rhs=DG, start=True, stop=True), reads=["DG", "onesf"], writes=[pk])
                            P.act(lambda e, ps=ps, ti=ti: e.activation(out=GE[:, ti * 128 - gs:(ti + 1) * 128 - gs], in_=ps[:, 0:128], func=AF.Identity),
                                  reads=[pk], writes=["GE"])
                    else:
                        w1v = D[p + "ffn_w1"].rearrange("(k p) n -> p k n", p=128)
                        w2v = D[p + "ffn_w2"]
                    for f in range(nf):
                        wb = w1b[cnt1[0] % 2]; wk = "w1b%d" % (cnt1[0] % 2); cnt1[0] += 1
                        P.dma("pool", wk + "g", lambda e, wb=wb, f=f, w1v=w1v: e.dma_start(out=wb[:, :, 0:128], in_=w1v[:, :, f * 128:(f + 1) * 128]), writes=[wk + "g"])
                        P.dma("pool", wk + "u", lambda e, wb=wb, f=f, w1v=w1v: e.dma_start(out=wb[:, :, 128:256], in_=w1v[:, :, fdim + f * 128:fdim + (f + 1) * 128]), writes=[wk + "u"])
                        for (s, n) in blks:
                            psg, pgk = bank()
                            psu, puk = bank()
                            for k in range(8):
                                P.pe(lambda e, psg=psg, k=k, wb=wb, s=s, n=n: e.matmul(psg[:, 0:n], lhsT=wb[:, k, 0:128], rhs=HT[:, k, s:s + n], start=(k == 0), stop=(k == 7)),
                                     reads=[wk + "g", "HT%d" % k], writes=[pgk])
                            for k in range(8):
                                P.pe(lambda e, psu=psu, k=k, wb=wb, s=s, n=n: e.matmul(psu[:, 0:n], lhsT=wb[:, k, 128:256], rhs=HT[:, k, s:s + n], start=(k == 0), stop=(k == 7)),
                                     reads=[wk + "u", "HT%d" % k], writes=[puk])
                            P.act(lambda e, psg=psg, n=n: e.activation(out=TS[:, 0:n], in_=psg[:, 0:n], func=AF.Silu), reads=[pgk], writes=["TS"])
                            if moe:
                                P.dve(lambda e, psu=psu, s=s, n=n: e.tensor_tensor(out=TU[:, 0:n], in0=psu[:, 0:n], in1=GE[:, s - gs:s - gs + n], op=ALU.mult),
                                      reads=[puk, "GE"], writes=["TU"])
                                P.dve(lambda e, f=f, s=s, n=n: e.tensor_tensor(out=ACT_[:, f, s - gs:s - gs + n], in0=TS[:, 0:n], in1=TU[:, 0:n], op=ALU.mult),
                                      reads=["TS", "TU"], writes=["ACT"])
                            else:
                                P.dve(lambda e, psu=psu, f=f, s=s, n=n: e.tensor_tensor(out=ACT_[:, f, s - gs:s - gs + n], in0=psu[:, 0:n], in1=TS[:, 0:n], op=ALU.mult),
                                      reads=[puk, "TS"], writes=["ACT"])
                    for m in range(8):
                        w2 = w2b[cnt2[0] % 2]; w2k = "w2b%d" % (cnt2[0] % 2); cnt2[0] += 1
                        P.dma("pool", w2k, lambda e, w2=w2, m=m, w2v=w2v: e.dma_start(out=w2, in_=w2v[:, m * 128:(m + 1) * 128].rearrange("(f p) n -> p f n", p=128)), writes=[w2k])
                        for (s, n) in blks:
                            ps, pk = bank()
                            for f in range(nf):
                                P.pe(lambda e, ps=ps, f=f, w2=w2, s=s, n=n: e.matmul(ps[:, 0:n], lhsT=w2[:, f, :], rhs=ACT_[:, f, s - gs:s - gs + n], start=(f == 0), stop=(f == nf - 1)),
                                     reads=[w2k, "ACT"], writes=[pk])
                            col = 1 if s < NCTX else 0
                            assert not (s < NCTX < s + n)
                            P.dve(lambda e, ps=ps, m=m, s=s, n=n, col=col: e.scalar_tensor_tensor(out=XT[:, m, s:s + n], in0=ps[:, 0:n], scalar=gate_ap(1, m, col),
                                                                                           in1=XT[:, m, s:s + n], op0=ALU.mult, op1=ALU.add),
                                  reads=[pk, "XT", "macc"], writes=["XT"])

        for l in layers:
            adaln(l)
            if l % 2 == 0:
                if not _DBG.get("skip_mix"):
                    even_mixer(l)
                if not _DBG.get("skip_ffn"):
                    ffn(l, [(0, 1280, [(0, 256), (256, 512), (768, 512)]), (1280, 1024, [(1280, 512), (1792, 512)])], moe=False)
            else:
                if not _DBG.get("skip_mix"):
                    try:
                        odd_mixer(l)
                    except _Stop:
                        pass
                if _DBG.get("skip_ffn"):
                    pass
                elif l == 3:
                    ffn(l, [(256, 768, [(256, 384), (640, 384)]), (1024, 768, [(1024, 384), (1408, 384)]),
                            (1792, 512, [(1792, 256), (2048, 256)])], moe=True)
                else:
                    ffn(l, [(0, 768, [(0, 256), (256, 512)]), (768, 768, [(768, 384), (1152, 384)]),
                            (1536, 768, [(1536, 384), (1920, 384)])], moe=True)
        P.barrier()
        if final:
            fg = smallp[:, 32:40]
            P.dma("sp", "fg", lambda e: e.dma_start(out=fg, in_=D["final_g"][:, :]), writes=["fg"])
            P.dve(lambda e: e.memset(AB[:, 0, :, :], 0.0), writes=["AB"])
            P.dve(lambda e: e.tensor_copy(out=AB[:, 0, :, 0], in_=fg), reads=["fg"], writes=["AB"])
            P.dve(lambda e: e.memset(AB[:, 1, :, :], 0.0), writes=["AB"])
            RS, _ = carve(0, [NT], F32)
            T0, _ = carve(NT * 4, [NT], F32)
            blks = ALLB
            bks = [bank() for _ in blks]
            for k in range(8):
                P.act(lambda e, k=k: e.activation(out=T0, in_=XT[:, k, :], func=AF.Square), reads=["XT"], writes=["T0"])
                for (ps, pk), (s, n) in zip(bks, blks):
                    P.pe(lambda e, ps=ps, s=s, n=n, k=k: e.matmul(ps[:, 0:n], lhsT=onesf[:], rhs=T0[:, s:s + n], start=(k == 0), stop=(k == 7)),
                         reads=["T0", "onesf"], writes=[pk])
            for (ps, pk), (s, n) in zip(bks, blks):
                P.act(lambda e, ps=ps, s=s, n=n: e.activation(out=RS[:, s:s + n], in_=ps[:, 0:n], func=AF.Sqrt, scale=1.0 / 1024, bias=epsb[:, 0:1]),
                      reads=[pk, "epsb"], writes=["RS"])
            P.dve(lambda e: e.reciprocal(out=RS, in_=RS), reads=["RS"], writes=["RS"])
            for k in range(8):
                P.dve(lambda e, k=k: e.scalar_tensor_tensor(out=XT[:, k, :], in0=XT[:, k, :], scalar=fg[:, k:k + 1], in1=RS, op0=ALU.mult, op1=ALU.mult),
                      reads=["XT", "RS", "fg"], writes=["XT"])
            tok = P.dma("sp", "out", lambda e: e.dma_start(out=out_d[:, :, :], in_=XT[:, :, NCTX:NT]), reads=["XT"])
        else:
            tok = P.dma("sp", "out", lambda e: e.dma_start(out=out_d[:, :, :], in_=XT[:]), reads=["XT"])
        P.emit(final_waits=[tok])
    return nc


def fm(v, nchunk):
    return np.ascontiguousarray(np.asarray(v, np.float32).reshape(nchunk, 128).T)


def rope_tables():
    n = 8
    inv = (10000.0 ** (-np.arange(n, dtype=np.float32) / n)).astype(np.float32)
    t = np.arange(2048)
    row = (t // 64).astype(np.float32); col = (t % 64).astype(np.float32)
    ang = np.concatenate([row[:, None] * inv, col[:, None] * inv], axis=-1).astype(np.float32)
    cos = np.cos(ang).astype(np.float32); sin = np.sin(ang).astype(np.float32)
    C = np.ones((96, NT), np.float32); S = np.zeros((96, NT), np.float32)
    cr, cc, sr, sc_ = cos[:, :8].T, cos[:, 8:].T, sin[:, :8].T, sin[:, 8:].T
    C[64:72, NCTX:] = cr; C[72:80, NCTX:] = cr; C[80:88, NCTX:] = cc; C[88:96, NCTX:] = cc
    S[64:72, NCTX:] = -sr; S[72:80, NCTX:] = sr; S[80:88, NCTX:] = -sc_; S[88:96, NCTX:] = sc_
    return np.ascontiguousarray(np.stack([C, S], axis=1))


SWAP = np.concatenate([np.arange(8, 16), np.arange(0, 8), np.arange(24, 32), np.arange(16, 24)])


def const_inputs():
    i = np.arange(128)
    m = np.zeros((128, 6, 128), np.float32)
    m[:, 0, :] = (i[:, None] <= i[None, :])
    m[:, 1, :] = (i[:, None] >= i[None, :])
    m[:, 2, :] = (i[:, None] > i[None, :])
    m[:, 3, :] = (i[:, None] < i[None, :])
    return {"ident": np.eye(128, dtype=np.float32), "masks": m, "rope": rope_tables()}


def layer_inputs(inp, l):
    p = "l%d_" % l
    d = {}
    g = lambda n: np.asarray(inp[p + n], np.float32)
    d[p + "mod_w"] = g("mod_w")
    d[p + "mod_b"] = fm(g("mod_b"), 48)
    d[p + "n1g"] = fm(g("norm1_g"), 8)
    d[p + "n2g"] = fm(g("norm2_g"), 8)
    d[p + "w_out"] = g("w_out")
    if l % 2 == 0:
        w_in = g("w_in")
        d[p + "w_in"] = w_in
        wkr = np.zeros((1024, 2, 96), np.float32)
        wkr[:, 0, 64:] = w_in[:, 1920:1952]
        wkr[:, 1, 64:] = w_in[:, 1920:1952][:, SWAP]
        d[p + "wkr"] = wkr
        d[p + "convw"] = np.ascontiguousarray(g("conv_w").reshape(3, 4, 128).transpose(2, 1, 0))
        d[p + "qng"] = fm(g("q_norm_g"), 2)
        d[p + "kvng"] = fm(g("kv_norm_g"), 1)
        wuq = g("w_uq")
        wsw = wuq.copy().reshape(256, 8, 96)
        wsw[:, :, 64:] = wsw[:, :, 64:][:, :, SWAP]
        d[p + "w_uq"] = np.ascontiguousarray(np.stack([wuq, wsw.reshape(256, 768)], axis=1))
        wukv = g("w_ukv").reshape(128, 8, 128)
        wuk = np.zeros((128, 8, 96), np.float32)
        wuk[:, :, :64] = wukv[:, :, :64]
        d[p + "w_uk"] = wuk
        d[p + "w_uv"] = np.ascontiguousarray(wukv[:, :, 64:].reshape(128, 512))
        d[p + "ffn_w1"] = g("ffn_w1")
        d[p + "ffn_w2"] = g("ffn_w2")
    else:
        d[p + "w_in"] = g("w_in")
        d[p + "convw"] = np.ascontiguousarray(g("qkv_conv_w").reshape(3, 24, 128).transpose(2, 1, 0))
        d[p + "alog"] = np.ascontiguousarray(np.broadcast_to(g("a_log").reshape(1, 16), (128, 16)))
        d[p + "dtb"] = np.ascontiguousarray(np.broadcast_to(g("dt_bias").reshape(1, 16), (128, 16)))
        d[p + "ong"] = np.ascontiguousarray(np.broadcast_to(g("o_norm_g").reshape(1, 128), (128, 128)))
        d[p + "router"] = g("router_w")
        d[p + "moe_w1"] = g("moe_w1")
        d[p + "moe_w2"] = g("moe_w2")
    return d


def core_inputs(inp, b):
    x = np.asarray(inp["x"][b], np.float32); ctx = np.asarray(inp["ctx"][b], np.float32)
    tok = np.concatenate([ctx, x], axis=0)
    xT = np.ascontiguousarray(tok.reshape(NT, 8, 128).transpose(2, 1, 0))
    cs = np.stack([np.asarray(inp["c"][b], np.float32), np.asarray(inp["c_ctx"], np.float32)], axis=-1)
    csT = np.ascontiguousarray(cs.reshape(8, 128, 2).transpose(1, 0, 2))
    return {"xT": xT, "csT": csT}


_ALL_INPUTS = (
    "x",
    "c",
    "ctx",
    "c_ctx",
    "l0_mod_w",
    "l0_mod_b",
    "l0_norm1_g",
    "l0_w_in",
    "l0_conv_w",
    "l0_q_norm_g",
    "l0_w_uq",
    "l0_kv_norm_g",
    "l0_w_ukv",
    "l0_w_out",
    "l0_norm2_g",
    "l0_ffn_w1",
    "l0_ffn_w2",
    "l1_mod_w",
    "l1_mod_b",
    "l1_norm1_g",
    "l1_w_in",
    "l1_qkv_conv_w",
    "l1_a_log",
    "l1_dt_bias",
    "l1_o_norm_g",
    "l1_w_out",
    "l1_norm2_g",
    "l1_router_w",
    "l1_moe_w1",
    "l1_moe_w2",
    "l2_mod_w",
    "l2_mod_b",
    "l2_norm1_g",
    "l2_w_in",
    "l2_conv_w",
    "l2_q_norm_g",
    "l2_w_uq",
    "l2_kv_norm_g",
    "l2_w_ukv",
    "l2_w_out",
    "l2_norm2_g",
    "l2_ffn_w1",
    "l2_ffn_w2",
    "l3_mod_w",
    "l3_mod_b",
    "l3_norm1_g",
    "l3_w_in",
    "l3_qkv_conv_w",
    "l3_a_log",
    "l3_dt_bias",
    "l3_o_norm_g",
    "l3_w_out",
    "l3_norm2_g",
    "l3_router_w",
    "l3_moe_w1",
    "l3_moe_w2",
    "final_norm_g",
)


_NC = {}


def kernel(**inputs):
    inputs = {n: inputs[n] for n in _ALL_INPUTS}
    layers = (0, 1, 2, 3)
    if "prog" not in _NC:
        _NC["prog"] = build(layers, final=True)
    nc = _NC["prog"]
    shared = const_inputs()
    shared["final_g"] = fm(inputs["final_norm_g"], 8)
    for l in layers:
        shared.update(layer_inputs(inputs, l))
    in_maps = []
    for b in range(8):
        d = dict(shared)
        d.update(core_inputs(inputs, b))
        in_maps.append(d)
    res = run_bass_kernel_spmd(nc, in_maps, core_ids=list(range(8)))
    out = np.empty((8, 2048, 1024), np.float32)
    for b in range(8):
        o = res.results[b]["outT"]
        out[b] = o.transpose(2, 1, 0).reshape(2048, 1024)
    return out
```

```python
import contextlib
import numpy as np
import concourse.bass as bass
import concourse.mybir as mybir
from concourse.bass_utils import run_bass_kernel_spmd

F32 = mybir.dt.float32
BF16 = mybir.dt.bfloat16
AF = mybir.ActivationFunctionType
ALU = mybir.AluOpType
AX = mybir.AxisListType

ENGS = ("pe", "act", "dve", "pool", "sp")
NT = 2304
NCTX = 256
EPS = 1e-6


class _Rec:
    def __init__(self):
        self.call = None

    def __getattr__(self, name):
        def f(*a, **kw):
            self.call = (name, a, kw)
            return self
        return f


class Prog:
    def __init__(self, nc, same_engine_sync=True):
        self.nc = nc
        self.q = {e: [] for e in ENGS}
        self.cnt = {e: 0 for e in ENGS}
        self.last_w = {}
        self.readers = {}
        self.dma_sems = {}
        self.last_tok = {}
        self.same_engine_sync = same_engine_sync
        self.btoks = []
        self.bar_fns = {}

    def op(self, eng, fn, reads=(), writes=(), dma=None, extra=()):
        deps = set(extra) | set(self.btoks)
        for k in reads:
            t = self.last_w.get(k)
            if t is not None:
                deps.add(t)
        for k in writes:
            t = self.last_w.get(k)
            if t is not None:
                deps.add(t)
            for r in self.readers.get(k, ()):
                deps.add(r)
        if dma is None:
            self.cnt[eng] += 1
            tok = (eng, self.cnt[eng])
        else:
            self.dma_sems[dma] = self.dma_sems.get(dma, 0) + 16
            tok = ("dma:" + dma, self.dma_sems[dma])
        r = _Rec()
        fn(r)
        self.q[eng].append((r.call, deps, tok))
        self.last_tok[tok[0]] = tok
        for k in reads:
            self.readers.setdefault(k, []).append(tok)
        for k in writes:
            self.last_w[k] = tok
            self.readers[k] = []
        return tok

    def pe(self, fn, reads=(), writes=()):
        return self.op("pe", fn, reads, writes)

    def act(self, fn, reads=(), writes=()):
        return self.op("act", fn, reads, writes)

    def dve(self, fn, reads=(), writes=()):
        return self.op("dve", fn, reads, writes)

    def dma(self, eng, slot, fn, reads=(), writes=()):
        return self.op(eng, fn, reads, writes, dma=slot)

    def barrier(self):
        toks = list(self.last_tok.values()) + list(self.btoks)
        nb = []
        for e in ("pe", "act", "dve", "pool"):
            nb.append(self.op(e, self.bar_fns[e], extra=toks))
        self.btoks = nb
        self.last_w = {}
        self.readers = {}

    def emit(self, final_waits=()):
        nc = self.nc
        sem_names = list(ENGS) + ["dma:" + s for s in self.dma_sems]
        with contextlib.ExitStack() as es:
            sems = {}
            for i, n in enumerate(sem_names):
                sems[n] = es.enter_context(nc.semaphore("s%d" % i))
            block = es.enter_context(nc.Block())
            handles = {"pe": "tensor", "act": "scalar", "dve": "vector", "pool": "gpsimd", "sp": "sync"}

            def make(engname):
                ops = self.q[engname]

                def body(eng):
                    seen = {}
                    for (fn, deps, tok) in ops:
                        need = {}
                        for (s, v) in deps:
                            if s == engname and (engname == "pe" or not self.same_engine_sync):
                                continue
                            if seen.get(s, 0) < v:
                                need[s] = max(need.get(s, 0), v)
                        for s, v in need.items():
                            eng.wait_ge(sems[s], v)
                            seen[s] = v
                        ins = getattr(eng, fn[0])(*fn[1], **fn[2])
                        ins.then_inc(sems[tok[0]], 16 if tok[0].startswith("dma:") else 1)
                    if engname == "sp":
                        for tok in final_waits:
                            eng.wait_ge(sems[tok[0]], tok[1])
                return body

            for engname in ENGS:
                if not self.q[engname] and engname != "sp":
                    continue
                getattr(block, handles[engname])(make(engname))


def blocks_of(start, length, maxw=512):
    out = []
    n = (length + maxw - 1) // maxw
    w = length // n
    assert w * n == length
    for i in range(n):
        out.append((start + i * w, w))
    return out


EVEN_IN = 1952
_DBG = {}


class _Stop(Exception):
    pass


def _step(name):
    lim = _DBG.get("astep")
    if lim is None:
        return
    _DBG["_cnt"] = _DBG.get("_cnt", 0) + 1
    if _DBG["_cnt"] >= lim:
        print("STOP at step", _DBG["_cnt"], name)
        raise _Stop()
ODD_IN = 4128


def build(layers=(0, 1, 2, 3), final=True):
    nc = bass.Bass("TRN2", target_bir_lowering=False)
    D = {}

    def din(name, shape, dt=F32):
        D[name] = nc.dram_tensor(name, list(shape), dt, kind="ExternalInput").ap()
        return D[name]

    din("xT", [128, 8, NT]); din("csT", [128, 8, 2])
    din("ident", [128, 128]); din("masks", [128, 6, 128]); din("rope", [96, 2, NT])
    din("final_g", [128, 8])
    for l in layers:
        p = "l%d_" % l
        din(p + "mod_w", [1024, 6144]); din(p + "mod_b", [128, 48]); din(p + "n1g", [128, 8]); din(p + "n2g", [128, 8])
        din(p + "w_out", [1024, 1024])
        if l % 2 == 0:
            din(p + "w_in", [1024, EVEN_IN]); din(p + "wkr", [1024, 2, 96]); din(p + "convw", [128, 4, 3])
            din(p + "qng", [128, 2]); din(p + "kvng", [128, 1]); din(p + "w_uq", [256, 2, 768])
            din(p + "w_uk", [128, 8, 96]); din(p + "w_uv", [128, 512])
            din(p + "ffn_w1", [1024, 5632]); din(p + "ffn_w2", [2816, 1024])
        else:
            din(p + "w_in", [1024, ODD_IN]); din(p + "convw", [128, 24, 3]); din(p + "alog", [128, 16]); din(p + "dtb", [128, 16])
            din(p + "ong", [128, 128]); din(p + "router", [1024, 8])
            din(p + "moe_w1", [8, 1024, 7168]); din(p + "moe_w2", [8, 3584, 1024])
    if final:
        out_d = nc.dram_tensor("outT", [128, 8, 2048], F32, kind="ExternalOutput").ap()
    else:
        out_d = nc.dram_tensor("outT", [128, 8, NT], F32, kind="ExternalOutput").ap()

    es = contextlib.ExitStack()
    with es:
        def sb(name, shape, dt):
            return es.enter_context(nc.sbuf_tensor(name, list(shape), dt))
        XT = sb("XT", [128, 8, NT], F32)
        HT = sb("HT", [128, 8, NT], BF16)
        ARB = 92 * 1024
        AR = sb("AR", [128, ARB // 4], F32)
        identf = sb("identf", [128, 128], F32)
        identb = sb("identb", [128, 128], BF16)
        scr = sb("scr", [128, 4], F32)
        onesf = sb("onesf", [128, 128], F32)
        onesb = sb("onesb", [128, 128], BF16)
        epsb = sb("epsb", [128, 1], F32)
        sc = sb("sc", [128, 8, 2], F32)
        macc = sb("macc", [128, 48, 2], F32)
        modb = sb("modb", [128, 48], F32)
        ng = sb("ng", [128, 2, 8], F32)
        AB = sb("ABm", [128, 4, 8, 2], F32)
        smallp = sb("smallp", [128, 64], F32)
        PS = [es.enter_context(nc.psum_tensor("ps%d" % i, [128, 512], F32)) for i in range(8)]
        P = Prog(nc, same_engine_sync=not _DBG.get("nosame", False))
        P.bar_fns = {
            "pe": lambda e: e.matmul(PS[7][0:1, 0:1], lhsT=identb[:, 0:1], rhs=identb[:, 0:1], start=True, stop=True),
            "act": lambda e: e.activation(out=scr[:, 0:1], in_=epsb[:, 0:1], func=AF.Identity),
            "dve": lambda e: e.memset(scr[:, 1:2], 0.0),
            "pool": lambda e: e.memset(scr[:, 2:3], 0.0),
        }
        psi = [0]

        def bank():
            i = psi[0] % 6
            psi[0] += 1
            return PS[i], "ps%d" % i

        def carve(off, shape, dt):
            n = int(np.prod(shape))
            bpe = 4 if dt == F32 else 2
            nby = n * bpe
            assert off % 4 == 0 and off + nby <= ARB, (off, nby, ARB)
            ap = AR[:, off // 4:(off + nby + 3) // 4]
            if dt != F32:
                ap = ap.bitcast(dt)[:, 0:n]
            if len(shape) == 2:
                ap = ap.rearrange("p (a b) -> p a b", b=shape[1])
            elif len(shape) == 3:
                ap = ap.rearrange("p (a b c) -> p a b c", b=shape[1], c=shape[2])
            return ap, off + ((nby + 3) // 4) * 4

        P.dma("sp", "c0", lambda e: e.dma_start(out=XT[:], in_=D["xT"][:, :, :]), writes=["XT"])
        P.dma("sp", "c1", lambda e: e.dma_start(out=identf[:], in_=D["ident"][:, :]), writes=["identf"])
        P.dma("sp", "c3", lambda e: e.dma_start(out=sc[:], in_=D["csT"][:, :, :]), writes=["sc"])
        P.dve(lambda e: e.memset(onesf[:], 1.0), writes=["onesf"])
        P.dve(lambda e: e.memset(onesb[:], 1.0), writes=["onesb"])
        P.dve(lambda e: e.memset(epsb[:], EPS), writes=["epsb"])
        P.dve(lambda e: e.tensor_copy(out=identb[:], in_=identf[:]), reads=["identf"], writes=["identb"])
        P.act(lambda e: e.activation(out=sc[:], in_=sc[:], func=AF.Silu), reads=["sc"], writes=["sc"])

        CTX = (0, NCTX)
        XS = (NCTX, NT - NCTX)

        def adaln(l):
            p = "l%d_" % l
            P.barrier()
            mw = [carve(0, [6144], F32)[0], carve(6144 * 4, [6144], F32)[0]]
            mwv = D[p + "mod_w"].rearrange("(k p) n -> p k n", p=128)
            P.dma("sp", "modb", lambda e: e.dma_start(out=modb[:], in_=D[p + "mod_b"][:, :]), writes=["modb"])
            P.dma("sp", "ng0", lambda e: e.dma_start(out=ng[:, 0, :], in_=D[p + "n1g"][:, :]), writes=["ng0"])
            P.dma("sp", "ng1", lambda e: e.dma_start(out=ng[:, 1, :], in_=D[p + "n2g"][:, :]), writes=["ng1"])
            for k in range(8):
                b = mw[k % 2]
                bk = "mw%d" % (k % 2)
                P.dma("sp", bk, lambda e, b=b, k=k: e.dma_start(out=b, in_=mwv[:, k, :]), writes=[bk])
                ps, pk = bank()
                for j in range(48):
                    P.pe(lambda e, j=j, b=b, ps=ps, k=k: e.matmul(ps[:, 2 * j:2 * j + 2], lhsT=b[:, j * 128:(j + 1) * 128],
                                                              rhs=sc[:, k, :], start=True, stop=True),
                         reads=[bk, "sc"], writes=[pk])
                mflat = macc[:].rearrange("p a b -> p (a b)")
                if k == 0:
                    P.dve(lambda e, ps=ps: e.tensor_copy(out=mflat, in_=ps[:, 0:96]), reads=[pk], writes=["macc"])
                else:
                    P.dve(lambda e, ps=ps: e.tensor_tensor(out=mflat, in0=mflat, in1=ps[:, 0:96], op=ALU.add),
                          reads=[pk, "macc"], writes=["macc"])
            P.dve(lambda e: e.tensor_tensor(out=macc[:], in0=macc[:], in1=modb[:, :, None].broadcast_to([128, 48, 2]), op=ALU.add),
                  reads=["macc", "modb"], writes=["macc"])
            for w in range(2):
                sh = macc[:, 24 * w:24 * w + 8, :]
                scl = macc[:, 24 * w + 8:24 * w + 16, :]
                P.dve(lambda e, w=w, scl=scl: e.scalar_tensor_tensor(
                    out=AB[:, 2 * w, :, :], in0=scl, scalar=1.0, in1=ng[:, w, :, None].broadcast_to([128, 8, 2]),
                    op0=ALU.add, op1=ALU.mult), reads=["macc", "ng%d" % w], writes=["AB"])
                P.dve(lambda e, w=w, sh=sh: e.tensor_copy(out=AB[:, 2 * w + 1, :, :], in_=sh), reads=["macc"], writes=["AB"])

        def gate_ap(w, k, col):
            return macc[:, 24 * w + 16 + k, col:col + 1]

        def norm_mod(w, rs_off, tmp_off, h32_cb=None):
            RS, _ = carve(rs_off, [NT], F32)
            T0, o1 = carve(tmp_off, [NT], F32)
            T1, _ = carve(o1, [NT], F32)
            tmps = [T0, T1]
            blks = blocks_of(0, NT - 256, 512) + [(NT - 256, 256)]
            bks = [bank() for _ in blks]
            for k in range(8):
                t = tmps[k % 2]; tk = "nt%d" % (k % 2)
                P.act(lambda e, t=t, k=k: e.activation(out=t, in_=XT[:, k, :], func=AF.Square), reads=["XT"], writes=[tk])
                for (ps, pk), (s, n) in zip(bks, blks):
                    P.pe(lambda e, ps=ps, t=t, s=s, n=n, k=k: e.matmul(ps[:, 0:n], lhsT=onesf[:], rhs=t[:, s:s + n],
                                                                    start=(k == 0), stop=(k == 7)),
                         reads=[tk, "onesf"], writes=[pk])
            for (ps, pk), (s, n) in zip(bks, blks):
                P.act(lambda e, ps=ps, s=s, n=n: e.activation(out=RS[:, s:s + n], in_=ps[:, 0:n], func=AF.Sqrt,
                                                              scale=1.0 / 1024, bias=epsb[:, 0:1]),
                      reads=[pk, "epsb"], writes=["RS"])
            P.dve(lambda e: e.reciprocal(out=RS, in_=RS), reads=["RS"], writes=["RS"])
            for k in range(8):
                t = tmps[k % 2]; tk = "nt%d" % (k % 2)
                P.dve(lambda e, t=t, k=k: e.tensor_tensor(out=t, in0=XT[:, k, :], in1=RS, op=ALU.mult),
                      reads=["XT", "RS"], writes=[tk])
                for col, (s, n) in ((1, CTX), (0, XS)):
                    if h32_cb is not None:
                        P.act(lambda e, t=t, k=k, col=col, s=s, n=n: e.activation(
                            out=t[:, s:s + n], in_=t[:, s:s + n], func=AF.Identity,
                            scale=AB[:, 2 * w, k, col:col + 1], bias=AB[:, 2 * w + 1, k, col:col + 1]),
                            reads=[tk, "AB"], writes=[tk])
                    else:
                        P.act(lambda e, t=t, k=k, col=col, s=s, n=n: e.activation(
                            out=HT[:, k, s:s + n], in_=t[:, s:s + n], func=AF.Identity,
                            scale=AB[:, 2 * w, k, col:col + 1], bias=AB[:, 2 * w + 1, k, col:col + 1]),
                            reads=[tk, "AB"], writes=["HT%d" % k])
                if h32_cb is not None:
                    P.dve(lambda e, t=t, k=k: e.tensor_copy(out=HT[:, k, :], in_=t), reads=[tk], writes=["HT%d" % k])
                    h32_cb(k, t, tk)

        HTK = ["HT%d" % k for k in range(8)]

        def linear(lhs_list, rhs_list, blks, evac, reads):
            for (s, n) in blks:
                ps, pk = bank()
                nk = len(lhs_list)
                for i in range(nk):
                    P.pe(lambda e, ps=ps, i=i, s=s, n=n: e.matmul(ps[:lhs_list[i].shape[-1], 0:n], lhsT=lhs_list[i],
                                                                  rhs=rhs_list[i](s, n), start=(i == 0), stop=(i == nk - 1)),
                         reads=reads, writes=[pk])
                evac(ps, pk, s, n)

        ALLB = blocks_of(0, 2048, 512) + [(2048, 256)]

        def even_mixer(l):
            p = "l%d_" % l
            P.barrier()
            off = 0
            RS, off = carve(off, [NT], F32)
            T0, off = carve(off, [NT], F32)
            T1, off = carve(off, [NT], F32)
            norm_mod(0, 0, NT * 4)
            T2 = RS
            wb, off = carve(off, [8, 384], BF16)
            wkr, off = carve(off, [8, 2, 96], BF16)
            att_end = off
            ROPE, off = carve(off, [2, NT], F32)
            SQ, _ = carve(off, [NT], F32)
            convy, off = carve(off, [4, NT], BF16)
            cqn, off = carve(off, [2, NT], BF16)
            ckvn, off = carve(off, [NT], BF16)
            krr, off = carve(off, [NT], BF16)
            cw = smallp[:, 0:12].rearrange("p (a b) -> p a b", b=3)
            qng = smallp[:, 12:14]
            kvng = smallp[:, 14:15]
            P.dma("sp", "sm0", lambda e: e.dma_start(out=cw, in_=D[p + "convw"][:, :, :]), writes=["cw"])
            P.dma("sp", "sm1", lambda e: e.dma_start(out=smallp[:, 12:14], in_=D[p + "qng"][:, :]), writes=["qng"])
            P.dma("sp", "sm2", lambda e: e.dma_start(out=smallp[:, 14:15], in_=D[p + "kvng"][:, :]), writes=["kvng"])
            P.dma("sp", "rope", lambda e: e.dma_start(out=ROPE[0:96], in_=D["rope"][:, :, :]), writes=["ROPE"])
            P.dma("pool", "wkr", lambda e: e.dma_start(out=wkr, in_=D[p + "wkr"].rearrange("(k p) a b -> p k a b", p=128)),
                  writes=["wkr"])
            winv = D[p + "w_in"].rearrange("(k p) n -> p k n", p=128)
            wk = "wb"

            def hrhs(k):
                return lambda s, n: HT[:, k, s:s + n]

            def ev_copy(dst, dk):
                def f(ps, pk, s, n):
                    P.act(lambda e: e.activation(out=dst[:, s:s + n], in_=ps[:, 0:n], func=AF.Identity), reads=[pk], writes=[dk])
                return f

            P.dma("pool", "wb", lambda e: e.dma_start(out=wb[:, :, 0:384], in_=winv[:, :, 1536:1920]), writes=[wk])
            rd = HTK + [wk]

            def feat_norm(chunks, nfeat, gain, dst_fn, dkey):
                raws = [T0, T1]
                for i, co in enumerate(chunks):
                    linear([wb[:, k, co:co + 128] for k in range(8)], [hrhs(k) for k in range(8)], ALLB, ev_copy(raws[i], "T%d" % i), rd)
                bks = [bank() for _ in ALLB]
                for i in range(len(chunks)):
                    P.act(lambda e, i=i: e.activation(out=SQ, in_=raws[i], func=AF.Square), reads=["T%d" % i], writes=["SQ"])
                    for (ps, pk), (s, n) in zip(bks, ALLB):
                        P.pe(lambda e, ps=ps, s=s, n=n, i=i: e.matmul(ps[:, 0:n], lhsT=onesf[:], rhs=SQ[:, s:s + n],
                                                                   start=(i == 0), stop=(i == len(chunks) - 1)),
                             reads=["SQ", "onesf"], writes=[pk])
                for (ps, pk), (s, n) in zip(bks, ALLB):
                    P.act(lambda e, ps=ps, s=s, n=n: e.activation(out=RS[:, s:s + n], in_=ps[:, 0:n], func=AF.Sqrt,
                                                                  scale=1.0 / nfeat, bias=epsb[:, 0:1]), reads=[pk, "epsb"], writes=["RS"])
                P.dve(lambda e: e.reciprocal(out=RS, in_=RS), reads=["RS"], writes=["RS"])
                for i in range(len(chunks)):
                    P.dve(lambda e, i=i: e.scalar_tensor_tensor(out=dst_fn(i), in0=raws[i], scalar=gain[:, i:i + 1], in1=RS,
                                                             op0=ALU.mult, op1=ALU.mult),
                          reads=["T%d" % i, "RS", "qng", "kvng"], writes=[dkey])
            feat_norm([0, 128], 256.0, qng, lambda i: cqn[:, i, :], "cqn")
            feat_norm([256], 128.0, kvng, lambda i: ckvn, "ckvn")
            def ev_kr0(ps, pk, s, n):
                P.dve(lambda e: e.tensor_tensor(out=T0[0:96, s:s + n], in0=ps[0:96, 0:n], in1=ROPE[0:96, 0, s:s + n], op=ALU.mult),
                      reads=[pk, "ROPE"], writes=["T0"])
            linear([wkr[:, k, 0, :] for k in range(8)], [hrhs(k) for k in range(8)], ALLB, ev_kr0, HTK + ["wkr"])
            def ev_kr1(ps, pk, s, n):
                P.dve(lambda e: e.tensor_tensor(out=T1[0:96, s:s + n], in0=ps[0:96, 0:n], in1=ROPE[0:96, 1, s:s + n], op=ALU.mult),
                      reads=[pk, "ROPE"], writes=["T1"])
                P.dve(lambda e: e.tensor_tensor(out=krr[0:96, s:s + n], in0=T0[0:96, s:s + n], in1=T1[0:96, s:s + n], op=ALU.add),
                      reads=["T0", "T1"], writes=["krr"])
            linear([wkr[:, k, 1, :] for k in range(8)], [hrhs(k) for k in range(8)], ALLB, ev_kr1, HTK + ["wkr"])
            for c in range(4):
                for j in range(3):
                    P.dma("pool", "wb", lambda e, j=j, c=c: e.dma_start(
                        out=wb[:, :, j * 128:(j + 1) * 128], in_=winv[:, :, j * 512 + c * 128:j * 512 + (c + 1) * 128]),
                        writes=[wk])
                linear([wb[:, k, 128:256] for k in range(8)], [hrhs(k) for k in range(8)], ALLB, ev_copy(T0, "T0"), HTK + [wk])
                def ev_mul(ps, pk, s, n):
                    P.dve(lambda e: e.tensor_tensor(out=T0[:, s:s + n], in0=ps[:, 0:n], in1=T0[:, s:s + n], op=ALU.mult),
                          reads=[pk, "T0"], writes=["T0"])
                linear([wb[:, k, 256:384] for k in range(8)], [hrhs(k) for k in range(8)], ALLB, ev_mul, HTK + [wk])
                linear([wb[:, k, 0:128] for k in range(8)], [hrhs(k) for k in range(8)], ALLB, ev_copy(T1, "T1"), HTK + [wk])
                P.dve(lambda e, c=c: e.tensor_scalar(out=T2, in0=T0, scalar1=cw[:, c, 1:2], scalar2=None, op0=ALU.mult),
                      reads=["T0", "cw"], writes=["T2"])
                for (s, n) in (CTX, XS):
                    P.dve(lambda e, c=c, s=s, n=n: e.scalar_tensor_tensor(out=T2[:, s + 1:s + n], in0=T0[:, s:s + n - 1], scalar=cw[:, c, 0:1],
                                                                       in1=T2[:, s + 1:s + n], op0=ALU.mult, op1=ALU.add),
                          reads=["T0", "T2", "cw"], writes=["T2"])
                    P.dve(lambda e, c=c, s=s, n=n: e.scalar_tensor_tensor(out=T2[:, s:s + n - 1], in0=T0[:, s + 1:s + n], scalar=cw[:, c, 2:3],
                                                                       in1=T2[:, s:s + n - 1], op0=ALU.mult, op1=ALU.add),
                          reads=["T0", "T2", "cw"], writes=["T2"])
                P.dve(lambda e, c=c: e.tensor_tensor(out=convy[:, c, :], in0=T2, in1=T1, op=ALU.mult),
                      reads=["T2", "T1", "SQ"], writes=["convy", "SQ"])

            P.barrier()
            a0 = 0
            wuq, a0 = carve(a0, [2, 2, 768], BF16)
            wuk, a0 = carve(a0, [8, 96], BF16)
            wuv, a0 = carve(a0, [512], BF16)
            Kh = []; Vh = []; Qh = []; PT = []
            for i in range(2):
                t, a0 = carve(a0, [NT], BF16); Kh.append(t)
            for i in range(2):
                t, a0 = carve(a0, [18, 64], BF16); Vh.append(t)
            for i in range(2):
                t, a0 = carve(a0, [512], BF16); Qh.append(t)
            for i in range(4):
                t, a0 = carve(a0, [512], BF16); PT.append(t)
            RC, a0 = carve(a0, [512], F32)
            QT, a0 = carve(a0, [512], F32)
            assert a0 <= att_end, (a0, att_end)
            P.dma("pool", "wuq", lambda e: e.dma_start(out=wuq, in_=D[p + "w_uq"].rearrange("(k p) a n -> p k a n", p=128)), writes=["wuq"])
            P.dma("pool", "wuk", lambda e: e.dma_start(out=wuk, in_=D[p + "w_uk"][:, :, :]), writes=["wuk"])
            P.dma("pool", "wuv", lambda e: e.dma_start(out=wuv, in_=D[p + "w_uv"][:, :]), writes=["wuv"])
            scale = 96.0 ** -0.5
            pti = [0]
            for h in range(8):
                K = Kh[h % 2]; kk = "K%d" % (h % 2)
                V = Vh[h % 2]; vk = "V%d" % (h % 2)
                def ev_k(ps, pk, s, n, K=K, kk=kk):
                    P.dve(lambda e: e.tensor_tensor(out=K[0:96, s:s + n], in0=ps[0:96, 0:n], in1=krr[0:96, s:s + n], op=ALU.add),
                          reads=[pk, "krr"], writes=[kk])
                linear([wuk[:, h, :]], [lambda s, n: ckvn[:, s:s + n]], ALLB, ev_k, ["wuk", "ckvn"])
                for g in range(3):
                    ps, pk = bank()
                    for t6 in range(6):
                        ti = g * 6 + t6
                        P.pe(lambda e, ps=ps, t6=t6, ti=ti, h=h: e.matmul(ps[:, t6 * 64:(t6 + 1) * 64], lhsT=ckvn[:, ti * 128:(ti + 1) * 128],
                                                                        rhs=wuv[:, h * 64:(h + 1) * 64], start=True, stop=True),
                             reads=["ckvn", "wuv"], writes=[pk])
                    P.act(lambda e, ps=ps, g=g, V=V: e.activation(out=V[:, g * 6:(g + 1) * 6, :],
                                                                 in_=ps[:, 0:384].rearrange("p (a b) -> p a b", b=64), func=AF.Identity),
                          reads=[pk], writes=[vk])
                qblocks = [(0, 256, 2)] + [(256 + i * 512, 512, 18) for i in range(4)]
                for qi, (qs, qn, nkt) in enumerate(qblocks):
                    Q = Qh[qi % 2]; qk = "Q%d" % (qi % 2)
                    ps0, pk0 = bank()
                    ps1, pk1 = bank()
                    for kc in range(2):
                        P.pe(lambda e, kc=kc, ps0=ps0: e.matmul(ps0[0:96, 0:qn], lhsT=wuq[:, kc, 0, h * 96:(h + 1) * 96], rhs=cqn[:, kc, qs:qs + qn],
                                                               start=(kc == 0), stop=(kc == 1)), reads=["wuq", "cqn"], writes=[pk0])
                    for kc in range(2):
                        P.pe(lambda e, kc=kc, ps1=ps1: e.matmul(ps1[0:96, 0:qn], lhsT=wuq[:, kc, 1, h * 96:(h + 1) * 96], rhs=cqn[:, kc, qs:qs + qn],
                                                               start=(kc == 0), stop=(kc == 1)), reads=["wuq", "cqn"], writes=[pk1])
                    P.dve(lambda e, ps0=ps0: e.tensor_tensor(out=QT[0:96, 0:qn], in0=ps0[0:96, 0:qn], in1=ROPE[0:96, 0, qs:qs + qn], op=ALU.mult),
                          reads=[pk0, "ROPE"], writes=["QT"])
                    P.dve(lambda e, ps1=ps1: e.tensor_tensor(out=RC[0:96, 0:qn], in0=ps1[0:96, 0:qn], in1=ROPE[0:96, 1, qs:qs + qn], op=ALU.mult),
                          reads=[pk1, "ROPE"], writes=["RC"])
                    P.dve(lambda e, Q=Q: e.tensor_tensor(out=Q[0:96, 0:qn], in0=QT[0:96, 0:qn], in1=RC[0:96, 0:qn], op=ALU.add),
                          reads=["QT", "RC"], writes=[qk])
                    pnum, pnk = PS[6], "ps6"
                    pden, pdk = PS[7], "ps7"
                    for kt in range(nkt):
                        pss, psk = bank()
                        P.pe(lambda e, pss=pss, kt=kt, K=K, Q=Q: e.matmul(pss[:, 0:qn], lhsT=K[0:96, kt * 128:(kt + 1) * 128], rhs=Q[0:96, 0:qn],
                                                                        start=True, stop=True), reads=[kk, qk], writes=[psk])
                        pt = PT[pti[0] % 4]; ptk = "PT%d" % (pti[0] % 4); pti[0] += 1
                        P.act(lambda e, pss=pss, pt=pt: e.activation(out=pt[:, 0:qn], in_=pss[:, 0:qn], func=AF.Exp, scale=scale),
                              reads=[psk], writes=[ptk])
                        P.pe(lambda e, pt=pt, kt=kt, V=V: e.matmul(pnum[0:64, 0:qn], lhsT=V[:, kt, :], rhs=pt[:, 0:qn],
                                                                  start=(kt == 0), stop=(kt == nkt - 1)), reads=[vk, ptk], writes=[pnk])
                        P.pe(lambda e, pt=pt, kt=kt: e.matmul(pden[0:64, 0:qn], lhsT=onesb[:, 0:64], rhs=pt[:, 0:qn],
                                                              start=(kt == 0), stop=(kt == nkt - 1)), reads=["onesb", ptk], writes=[pdk])
                    P.dve(lambda e: e.reciprocal(out=RC[0:64, 0:qn], in_=pden[0:64, 0:qn]), reads=[pdk], writes=["RC"])
                    P.dve(lambda e: e.tensor_tensor(out=HT[0:64, h, qs:qs + qn], in0=pnum[0:64, 0:qn], in1=RC[0:64, 0:qn], op=ALU.mult),
                          reads=[pnk, "RC"], writes=["HT%d" % h])
            P.barrier()
            a0 = 0
            wob = []
            for i in range(2):
                t, a0 = carve(a0, [12, 128], BF16); wob.append(t)
            wov = D[p + "w_out"]
            for m in range(8):
                wo = wob[m % 2]; wok = "wo%d" % (m % 2)
                P.dma("pool", wok + "a", lambda e, wo=wo, m=m: e.dma_start(
                    out=wo[:, 0:4, :], in_=wov[0:512, m * 128:(m + 1) * 128].rearrange("(k p) n -> p k n", p=128)), writes=[wok + "a"])
                P.dma("pool", wok + "b", lambda e, wo=wo, m=m: e.dma_start(
                    out=wo[0:64, 4:12, :], in_=wov[512:1024, m * 128:(m + 1) * 128].rearrange("(k p) n -> p k n", p=64)), writes=[wok + "b"])
                lhs = [wo[:, c, :] for c in range(4)] + [wo[0:64, 4 + h, :] for h in range(8)]
                rhs = [(lambda c: lambda s, n: convy[:, c, s:s + n])(c) for c in range(4)] + \
                      [(lambda h: lambda s, n: HT[0:64, h, s:s + n])(h) for h in range(8)]
                for col, seg in ((1, [(0, 256)]), (0, blocks_of(256, 2048, 512))):
                    def ev(ps, pk, s, n, m=m, col=col):
                        P.dve(lambda e: e.scalar_tensor_tensor(out=XT[:, m, s:s + n], in0=ps[:, 0:n], scalar=gate_ap(0, m, col),
                                                               in1=XT[:, m, s:s + n], op0=ALU.mult, op1=ALU.add),
                              reads=[pk, "XT", "macc"], writes=["XT"])
                    linear(lhs, rhs, seg, ev, ["convy", wok + "a", wok + "b"] + HTK)

        def odd_mixer(l):
            p = "l%d_" % l
            P.barrier()
            off = 0
            RS, off = carve(off, [NT], F32)
            T0, off = carve(off, [NT], F32)
            T1, off = carve(off, [NT], F32)
            norm_mod(0, 0, NT * 4)
            tmp_end = off
            masks, off = carve(off, [6, 128], F32)
            ABr, off = carve(off, [18, 32], F32)
            Gt, off = carve(off, [18, 16], F32)
            BE, off = carve(off, [18, 16], F32)
            NBE, off = carve(off, [18, 16], F32)
            GC, off = carve(off, [18, 32], F32)
            EG, off = carve(off, [18, 16], F32)
            NEG, off = carve(off, [18, 16], F32)
            EKD, off = carve(off, [18, 16], F32)
            GEND, off = carve(off, [18, 16], F32)
            alog, off = carve(off, [16], F32)
            dtb, off = carve(off, [16], F32)
            ong, off = carve(off, [128], F32)
            cwo, off = carve(off, [24, 3], F32)
            one1, off = carve(off, [1], F32)
            S_, off = carve(off, [128], F32)
            Sbf, off = carve(off, [128], BF16)
            wab, off = carve(off, [8, 32], BF16)
            qT, off = carve(off, [NT], BF16)
            kT, off = carve(off, [NT], BF16)
            vT = None
            ktm, off = carve(off, [18, 128], BF16)
            vtm, off = carve(off, [18, 128], BF16)
            Zst, off = carve(off, [18, 128], BF16)
            vTa, _ = carve(off, [NT], BF16)
            AT, off = carve(off, [18, 128], BF16)
            Oacc, off = carve(off, [18, 128], F32)
            whq, off = carve(off, [8, 512], BF16)
            woh, off = carve(off, [1024], BF16)
            t0 = 0
            Gr, t0 = carve(t0, [4, 128], F32)
            E_, t0 = carve(t0, [4, 128], F32)
            Em, t0 = carve(t0, [4, 128], F32)
            EmS, t0 = carve(t0, [4, 128], F32)
            Pb = []; Qb = []
            for i in range(2):
                t, t0 = carve(t0, [4, 128], F32); Pb.append(t)
            for i in range(2):
                t, t0 = carve(t0, [4, 128], F32); Qb.append(t)
            Zw, t0 = carve(t0, [4, 128], F32)
            rr, t0 = carve(t0, [128], BF16)
            vnew, t0 = carve(t0, [128], BF16)
            kd, t0 = carve(t0, [128], BF16)
            tmpo, t0 = carve(t0, [128], F32)
            tmp2, t0 = carve(t0, [128], F32)
            c0 = 0
            SQc, c0 = carve(c0, [18, 128], F32)
            ssb, c0 = carve(c0, [18], F32)
            ytm, c0 = carve(c0, [18, 128], BF16)
            yT, c0 = carve(c0, [NT], BF16)
            assert c0 <= tmp_end, (c0, tmp_end)
            winv = D[p + "w_in"].rearrange("(k p) n -> p k n", p=128)
            P.dma("sp", "o0", lambda e: e.dma_start(out=masks, in_=D["masks"][:, :, :]), writes=["masks"])
            P.dma("sp", "o1", lambda e: e.dma_start(out=alog, in_=D[p + "alog"][:, :]), writes=["alog"])
            P.dma("sp", "o2", lambda e: e.dma_start(out=dtb, in_=D[p + "dtb"][:, :]), writes=["dtb"])
            P.dma("sp", "o3", lambda e: e.dma_start(out=ong, in_=D[p + "ong"][:, :]), writes=["ong"])
            P.dma("sp", "o4", lambda e: e.dma_start(out=cwo, in_=D[p + "convw"][:, :, :]), writes=["cwo"])
            P.dma("pool", "wab", lambda e: e.dma_start(out=wab, in_=winv[:, :, 4096:4128]), writes=["wab"])
            P.dve(lambda e: e.memset(one1, 1.0), writes=["one1"])
            for g0 in range(0, 18, 16):
                ps, pk = bank()
                tl = list(range(g0, min(18, g0 + 16)))
                for ti in tl:
                    for k in range(8):
                        P.pe(lambda e: e.matmul(ps[:, (ti - g0) * 32:(ti - g0 + 1) * 32], lhsT=HT[:, k, ti * 128:(ti + 1) * 128], rhs=wab[:, k, :],
                                                start=(k == 0), stop=(k == 7)), reads=["HT%d" % k, "wab"], writes=[pk])
                P.act(lambda e: e.activation(out=ABr[:, g0:g0 + len(tl), :], in_=ps[:, 0:32 * len(tl)].rearrange("p (a b) -> p a b", b=32), func=AF.Identity),
                      reads=[pk], writes=["ABr"])
            b16 = lambda a: a[:, None, :].broadcast_to([128, 18, 16])
            P.dve(lambda e: e.tensor_tensor(out=Gt, in0=ABr[:, :, 0:16], in1=b16(dtb), op=ALU.add), reads=["ABr", "dtb"], writes=["Gt"])
            P.act(lambda e: e.activation(out=Gt, in_=Gt, func=AF.Exp), reads=["Gt"], writes=["Gt"])
            P.act(lambda e: e.activation(out=Gt, in_=Gt, func=AF.Ln, bias=one1[:, 0:1], scale=1.0), reads=["Gt", "one1"], writes=["Gt"])
            P.act(lambda e: e.activation(out=alog, in_=alog, func=AF.Exp), reads=["alog"], writes=["alog"])
            P.dve(lambda e: e.scalar_tensor_tensor(out=Gt, in0=Gt, scalar=-1.0, in1=b16(alog), op0=ALU.mult, op1=ALU.mult), reads=["Gt", "alog"], writes=["Gt"])
            P.act(lambda e: e.activation(out=BE, in_=ABr[:, :, 16:32], func=AF.Sigmoid), reads=["ABr"], writes=["BE"])
            P.dve(lambda e: e.tensor_scalar(out=NBE, in0=BE, scalar1=-1.0, scalar2=None, op0=ALU.mult), reads=["BE"], writes=["NBE"])
            for g0 in range(0, 18, 16):
                ps, pk = bank()
                tl = list(range(g0, min(18, g0 + 16)))
                for ti in tl:
                    c = (ti - g0) * 32
                    P.pe(lambda e: e.matmul(ps[:, c:c + 8], lhsT=masks[:, 0, :], rhs=Gt[:, ti, 0:8], start=True, stop=True), reads=["masks", "Gt"], writes=[pk])
                    P.pe(lambda e: e.matmul(ps[:, c + 8:c + 16], lhsT=masks[:, 1, :], rhs=Gt[:, ti, 8:16], start=True, stop=True), reads=["masks", "Gt"], writes=[pk])
                    P.pe(lambda e: e.matmul(ps[:, c + 16:c + 32], lhsT=onesf[:], rhs=Gt[:, ti, :], start=True, stop=True), reads=["onesf", "Gt"], writes=[pk])
                P.act(lambda e: e.activation(out=GC[:, g0:g0 + len(tl), :], in_=ps[:, 0:32 * len(tl)].rearrange("p (a b) -> p a b", b=32), func=AF.Identity),
                      reads=[pk], writes=["GC"])
            P.act(lambda e: e.activation(out=EG, in_=GC[:, :, 0:16], func=AF.Exp), reads=["GC"], writes=["EG"])
            P.dve(lambda e: e.tensor_scalar(out=NEG, in0=EG, scalar1=-1.0, scalar2=None, op0=ALU.mult), reads=["EG"], writes=["NEG"])
            P.dve(lambda e: e.tensor_tensor(out=EKD, in0=GC[:, :, 16:32], in1=GC[:, :, 0:16], op=ALU.subtract), reads=["GC"], writes=["EKD"])
            P.act(lambda e: e.activation(out=EKD, in_=EKD, func=AF.Exp), reads=["EKD"], writes=["EKD"])
            P.act(lambda e: e.activation(out=GEND, in_=GC[:, :, 16:32], func=AF.Exp), reads=["GC"], writes=["GEND"])
            if _DBG.get("stop_pre"):
                fl = lambda a: a.rearrange("p a b -> p (a b)")
                for src_, k_, o_ in ((Gt, 0, 0), (BE, 0, 288), (GC, 1, 0), (EG, 2, 0), (EKD, 2, 288), (GEND, 3, 0), (ABr, 4, 0)):
                    n_ = src_.shape[1] * src_.shape[2]
                    P.dve(lambda e: e.tensor_copy(out=XT[:, k_, o_:o_ + n_], in_=fl(src_)), reads=["Gt", "BE", "GC", "EG", "EKD", "GEND", "ABr"], writes=["XT"])
                return

            def hrhs(k):
                return lambda s, n: HT[:, k, s:s + n]
            tiles4 = [list(range(i, min(18, i + 4))) for i in range(0, 18, 4)]
            for h in range(8):
                P.barrier()
                for j in range(4):
                    P.dma("pool", "whq%d" % j, lambda e: e.dma_start(out=whq[:, :, j * 128:(j + 1) * 128],
                                                                   in_=winv[:, :, j * 1024 + h * 128:j * 1024 + (h + 1) * 128]), writes=["whq%d" % j])
                _step("whq dma")
                P.dma("pool", "woh", lambda e: e.dma_start(out=woh, in_=D[p + "w_out"][h * 128:(h + 1) * 128, :]), writes=["woh"])
                _step("woh dma")
                for j, dst, dk in ((0, qT, "qT"), (1, kT, "kT"), (2, vTa, "vTa")):
                    ci = j * 8 + h
                    def ev(ps, pk, s, n):
                        P.act(lambda e: e.activation(out=T0[:, s:s + n], in_=ps[:, 0:n], func=AF.Identity), reads=[pk], writes=["T0"])
                    linear([whq[:, k, j * 128:(j + 1) * 128] for k in range(8)], [hrhs(k) for k in range(8)], ALLB, ev, HTK + ["whq%d" % j])
                    _step("proj %d" % j)
                    P.dve(lambda e: e.tensor_scalar(out=T1, in0=T0, scalar1=cwo[:, ci, 1:2], scalar2=None, op0=ALU.mult), reads=["T0", "cwo"], writes=["T1"])
                    for (s, n) in (CTX, XS):
                        P.dve(lambda e: e.scalar_tensor_tensor(out=T1[:, s + 1:s + n], in0=T0[:, s:s + n - 1], scalar=cwo[:, ci, 0:1],
                                                               in1=T1[:, s + 1:s + n], op0=ALU.mult, op1=ALU.add), reads=["T0", "T1", "cwo"], writes=["T1"])
                        P.dve(lambda e: e.scalar_tensor_tensor(out=T1[:, s:s + n - 1], in0=T0[:, s + 1:s + n], scalar=cwo[:, ci, 2:3],
                                                               in1=T1[:, s:s + n - 1], op0=ALU.mult, op1=ALU.add), reads=["T0", "T1", "cwo"], writes=["T1"])
                    _step("conv %d" % j)
                    if j == 2:
                        P.act(lambda e: e.activation(out=dst, in_=T1, func=AF.Silu), reads=["T1"], writes=[dk])
                    else:
                        P.act(lambda e: e.activation(out=T1, in_=T1, func=AF.Silu), reads=["T1"], writes=["T1"])
                        P.act(lambda e: e.activation(out=T0, in_=T1, func=AF.Square), reads=["T1", "T0"], writes=["T0"])
                        for (s, n) in ALLB:
                            ps, pk = bank()
                            P.pe(lambda e: e.matmul(ps[:, 0:n], lhsT=onesf[:], rhs=T0[:, s:s + n], start=True, stop=True), reads=["T0", "onesf"], writes=[pk])
                            P.act(lambda e: e.activation(out=RS[:, s:s + n], in_=ps[:, 0:n], func=AF.Sqrt, scale=1.0, bias=epsb[:, 0:1]), reads=[pk, "epsb"], writes=["RS"])
                        P.dve(lambda e: e.reciprocal(out=RS, in_=RS), reads=["RS"], writes=["RS"])
                        sc_ = (128.0 ** -0.5) if j == 0 else 1.0
                        P.dve(lambda e: e.scalar_tensor_tensor(out=dst, in0=T1, scalar=sc_, in1=RS, op0=ALU.mult, op1=ALU.mult), reads=["T1", "RS"], writes=[dk])
                    _step("l2norm %d" % j)
                _step("A proj done")
                for src, sk, dstm, dmk in ((kT, "kT", ktm, "ktm"), (vTa, "vTa", vtm, "vtm")):
                    for tl in tiles4:
                        ps, pk = bank()
                        for jj, ti in enumerate(tl):
                            P.pe(lambda e: e.matmul(ps[:, jj * 128:(jj + 1) * 128], lhsT=src[:, ti * 128:(ti + 1) * 128], rhs=identb[:], start=True, stop=True),
                                 reads=[sk, "identb"], writes=[pk])
                        P.act(lambda e: e.activation(out=dstm[:, tl[0]:tl[0] + len(tl), :], in_=ps[:, 0:128 * len(tl)].rearrange("p (a b) -> p a b", b=128), func=AF.Identity),
                              reads=[pk], writes=[dmk])
                    _step("transposes " + dmk)
                if _DBG.get("stop_A") and h == _DBG.get("dbg_head", 0):
                    P.dve(lambda e: e.tensor_copy(out=XT[:, 0, :], in_=qT), reads=["qT"], writes=["XT"])
                    P.dve(lambda e: e.tensor_copy(out=XT[:, 1, :], in_=kT), reads=["kT"], writes=["XT"])
                    P.dve(lambda e: e.tensor_copy(out=XT[:, 2, :], in_=vtm.rearrange("p a b -> p (a b)")), reads=["vtm"], writes=["XT"])
                    P.dve(lambda e: e.tensor_copy(out=XT[:, 3, :], in_=ktm.rearrange("p a b -> p (a b)")), reads=["ktm"], writes=["XT"])
                    return
                P.barrier()
                for dr in range(2):
                    col = dr * 8 + h
                    mg, mdt, mincl, mstr = (0, 2, 0, 3) if dr == 0 else (1, 3, 1, 2)
                    for tl in tiles4:
                        nt4 = len(tl); t0_ = tl[0]; W = nt4 * 128
                        bcm = lambda mi: masks[:, mi, :][:, None, :].broadcast_to([128, nt4, 128])
                        gsl = Gt[:, t0_:t0_ + nt4, col:col + 1].broadcast_to([128, nt4, 128])
                        P.dve(lambda e: e.tensor_tensor(out=Gr[:, 0:nt4, :], in0=bcm(mg), in1=gsl, op=ALU.mult), reads=["masks", "Gt"], writes=["Gr"])
                        ps, pk = bank()
                        P.pe(lambda e: e.matmul(ps[:, 0:W], lhsT=masks[:, mdt, :], rhs=Gr[:, 0:nt4, :].rearrange("p a b -> p (a b)"), start=True, stop=True),
                             reads=["masks", "Gr"], writes=[pk])
                        P.act(lambda e: e.activation(out=E_[:, 0:nt4, :], in_=ps[:, 0:W].rearrange("p (a b) -> p a b", b=128), func=AF.Exp), reads=[pk], writes=["E_"])
                        P.dve(lambda e: e.tensor_tensor(out=Em[:, 0:nt4, :], in0=E_[:, 0:nt4, :], in1=bcm(mincl), op=ALU.mult), reads=["E_", "masks"], writes=["Em"])
                        P.dve(lambda e: e.tensor_tensor(out=EmS[:, 0:nt4, :], in0=E_[:, 0:nt4, :], in1=bcm(mstr), op=ALU.mult), reads=["E_", "masks"], writes=["EmS"])
                        nb_ = NBE[:, t0_:t0_ + nt4, col:col + 1].broadcast_to([128, nt4, 128])
                        P.dve(lambda e: e.tensor_tensor(out=EmS[:, 0:nt4, :], in0=EmS[:, 0:nt4, :], in1=nb_, op=ALU.mult), reads=["EmS", "NBE"], writes=["EmS"])
                        pkk, pkkk = bank()
                        pkq, pkqk = bank()
                        for jj, ti in enumerate(tl):
                            P.pe(lambda e: e.matmul(pkk[:, jj * 128:(jj + 1) * 128], lhsT=kT[:, ti * 128:(ti + 1) * 128], rhs=kT[:, ti * 128:(ti + 1) * 128], start=True, stop=True),
                                 reads=["kT"], writes=[pkkk])
                            P.pe(lambda e: e.matmul(pkq[:, jj * 128:(jj + 1) * 128], lhsT=kT[:, ti * 128:(ti + 1) * 128], rhs=qT[:, ti * 128:(ti + 1) * 128], start=True, stop=True),
                                 reads=["kT", "qT"], writes=[pkqk])
                        r3 = lambda a: a[:, 0:W].rearrange("p (a b) -> p a b", b=128)
                        P.dve(lambda e: e.tensor_tensor(out=AT[:, t0_:t0_ + nt4, :], in0=r3(pkq), in1=Em[:, 0:nt4, :], op=ALU.mult), reads=[pkqk, "Em"], writes=["AT"])
                        halves = [(0, min(2, nt4))] + ([(2, nt4)] if nt4 > 2 else [])
                        hk = lambda base, hi: "%sh%d" % (base, hi)
                        allh = lambda base: [hk(base, hi) for hi in range(len(halves))]
                        r3h = lambda a_, lo, hi_: a_[:, lo * 128:hi_ * 128].rearrange("p (a b) -> p a b", b=128)
                        Pc, Qc = Pb[0], Qb[0]
                        P.dve(lambda e: e.tensor_tensor(out=Pc[:, 0:nt4, :], in0=r3(pkk), in1=EmS[:, 0:nt4, :], op=ALU.mult), reads=[pkkk, "EmS"], writes=allh("Pb0"))
                        for hi, (lo, up) in enumerate(halves):
                            ps, pk = bank()
                            for jj in range(lo, up):
                                P.pe(lambda e: e.matmul(ps[:, jj * 128:(jj + 1) * 128], lhsT=Pc[:, jj, :], rhs=identf[:], start=True, stop=True), reads=[hk("Pb0", hi), "identf"], writes=[pk])
                            P.act(lambda e: e.activation(out=Qc[:, lo:up, :], in_=r3h(ps, lo, up), func=AF.Identity), reads=[pk], writes=[hk("Qb0", hi)])
                            P.dve(lambda e: e.tensor_tensor(out=Zw[:, lo:up, :], in0=Pc[:, lo:up, :], in1=identf[:][:, None, :].broadcast_to([128, up - lo, 128]), op=ALU.add),
                                  reads=[hk("Pb0", hi), "identf"], writes=[hk("Zw", hi)])
                        cur = 0
                        for it in range(1, 7):
                            nx = 1 - cur
                            Pc, Qc, Pn, Qn = Pb[cur], Qb[cur], Pb[nx], Qb[nx]
                            pck, qck, pnk_, qnk = "Pb%d" % cur, "Qb%d" % cur, "Pb%d" % nx, "Qb%d" % nx
                            for hi, (lo, up) in enumerate(halves):
                                ps, pk = bank()
                                for jj in range(lo, up):
                                    P.pe(lambda e: e.matmul(ps[:, jj * 128:(jj + 1) * 128], lhsT=Pc[:, jj, :], rhs=Qc[:, jj, :], start=True, stop=True), reads=[hk(pck, hi), hk(qck, hi)], writes=[pk])
                                P.act(lambda e: e.activation(out=Qn[:, lo:up, :], in_=r3h(ps, lo, up), func=AF.Identity), reads=[pk], writes=[hk(qnk, hi)])
                            if it < 6:
                                for hi, (lo, up) in enumerate(halves):
                                    ps2, pk2 = bank()
                                    for jj in range(lo, up):
                                        P.pe(lambda e: e.matmul(ps2[:, jj * 128:(jj + 1) * 128], lhsT=Qc[:, jj, :], rhs=Pc[:, jj, :], start=True, stop=True), reads=[hk(pck, hi), hk(qck, hi)], writes=[pk2])
                                    P.act(lambda e: e.activation(out=Pn[:, lo:up, :], in_=r3h(ps2, lo, up), func=AF.Identity), reads=[pk2], writes=[hk(pnk_, hi)])
                            for hi, (lo, up) in enumerate(halves):
                                ps3, pk3 = bank()
                                for jj in range(lo, up):
                                    P.pe(lambda e: e.matmul(ps3[:, jj * 128:(jj + 1) * 128], lhsT=Qn[:, jj, :], rhs=Zw[:, jj, :], start=True, stop=True), reads=[hk(qnk, hi), hk("Zw", hi)], writes=[pk3])
                                dstz = Zst[:, t0_ + lo:t0_ + up, :] if it == 6 else Zw[:, lo:up, :]
                                P.dve(lambda e: e.tensor_tensor(out=dstz, in0=r3h(ps3, lo, up), in1=Zw[:, lo:up, :], op=ALU.add), reads=[pk3, hk("Zw", hi)],
                                      writes=["Zst", hk("Zw", hi)] if it == 6 else [hk("Zw", hi)])
                            cur = nx
                    if _DBG.get("stop_chain"):
                        return
                    order = list(range(18)) if dr == 0 else [1, 0] + list(range(17, 1, -1))
                    P.dve(lambda e: e.memset(S_, 0.0), writes=["S"])
                    P.dve(lambda e: e.memset(Sbf, 0.0), writes=["Sbf"])
                    for ti in order:
                        tsl = slice(ti * 128, (ti + 1) * 128)
                        psA, pka = bank()
                        P.pe(lambda e: e.matmul(psA[:, 0:128], lhsT=kT[:, tsl], rhs=Sbf, start=True, stop=True), reads=["kT", "Sbf"], writes=[pka])
                        P.pe(lambda e: e.matmul(psA[:, 128:256], lhsT=qT[:, tsl], rhs=Sbf, start=True, stop=True), reads=["qT", "Sbf"], writes=[pka])
                        P.dve(lambda e: e.scalar_tensor_tensor(out=rr, in0=psA[:, 0:128], scalar=NEG[:, ti, col:col + 1], in1=vtm[:, ti, :], op0=ALU.mult, op1=ALU.add),
                              reads=[pka, "NEG", "vtm"], writes=["rr"])
                        P.dve(lambda e: e.tensor_scalar(out=tmpo, in0=psA[:, 128:256], scalar1=EG[:, ti, col:col + 1], scalar2=None, op0=ALU.mult), reads=[pka, "EG"], writes=["tmpo"])
                        psB, pkb = bank()
                        P.pe(lambda e: e.matmul(psB[:, 0:128], lhsT=Zst[:, ti, :], rhs=rr, start=True, stop=True), reads=["Zst", "rr"], writes=[pkb])
                        P.dve(lambda e: e.tensor_scalar(out=vnew, in0=psB[:, 0:128], scalar1=BE[:, ti, col:col + 1], scalar2=None, op0=ALU.mult), reads=[pkb, "BE"], writes=["vnew"])
                        P.dve(lambda e: e.tensor_scalar(out=kd, in0=ktm[:, ti, :], scalar1=EKD[:, ti, col:col + 1], scalar2=None, op0=ALU.mult), reads=["ktm", "EKD"], writes=["kd"])
                        psC, pkc = bank()
                        P.pe(lambda e: e.matmul(psC[:, 0:128], lhsT=AT[:, ti, :], rhs=vnew, start=True, stop=True), reads=["AT", "vnew"], writes=[pkc])
                        P.pe(lambda e: e.matmul(psC[:, 128:256], lhsT=kd, rhs=vnew, start=True, stop=True), reads=["kd", "vnew"], writes=[pkc])
                        if dr == 0:
                            P.dve(lambda e: e.tensor_tensor(out=Oacc[:, ti, :], in0=psC[:, 0:128], in1=tmpo, op=ALU.add), reads=[pkc, "tmpo"], writes=["Oacc"])
                        else:
                            P.dve(lambda e: e.tensor_tensor(out=tmp2, in0=psC[:, 0:128], in1=tmpo, op=ALU.add), reads=[pkc, "tmpo"], writes=["tmp2"])
                            P.dve(lambda e: e.tensor_tensor(out=Oacc[:, ti, :], in0=Oacc[:, ti, :], in1=tmp2, op=ALU.add), reads=["tmp2", "Oacc"], writes=["Oacc"])
                        P.dve(lambda e: e.scalar_tensor_tensor(out=S_, in0=S_, scalar=GEND[:, ti, col:col + 1], in1=psC[:, 128:256], op0=ALU.mult, op1=ALU.add),
                              reads=[pkc, "S", "GEND"], writes=["S"])
                        P.act(lambda e: e.activation(out=Sbf, in_=S_, func=AF.Identity), reads=["S"], writes=["Sbf"])
                if _DBG.get("stop_B") and h == _DBG.get("dbg_head", 0):
                    P.dve(lambda e: e.tensor_copy(out=XT[:, 0, :], in_=Oacc.rearrange("p a b -> p (a b)")), reads=["Oacc"], writes=["XT"])
                    P.dve(lambda e: e.tensor_copy(out=XT[:, 1, :], in_=Zst.rearrange("p a b -> p (a b)")), reads=["Zst"], writes=["XT"])
                    P.dve(lambda e: e.tensor_copy(out=XT[:, 2, :], in_=AT.rearrange("p a b -> p (a b)")), reads=["AT"], writes=["XT"])
                    return
                P.barrier()
                P.dve(lambda e: e.tensor_tensor(out=SQc, in0=Oacc, in1=Oacc, op=ALU.mult), reads=["Oacc"], writes=["SQc"])
                P.dve(lambda e: e.tensor_reduce(out=ssb, in_=SQc, axis=AX.X, op=ALU.add), reads=["SQc"], writes=["ssb"])
                P.act(lambda e: e.activation(out=ssb, in_=ssb, func=AF.Sqrt, scale=1.0 / 128, bias=epsb[:, 0:1]), reads=["ssb", "epsb"], writes=["ssb"])
                P.dve(lambda e: e.reciprocal(out=ssb, in_=ssb), reads=["ssb"], writes=["ssb"])
                P.dve(lambda e: e.tensor_tensor(out=Oacc, in0=Oacc, in1=ssb[:, :, None].broadcast_to([128, 18, 128]), op=ALU.mult), reads=["Oacc", "ssb"], writes=["Oacc"])
                P.dve(lambda e: e.tensor_tensor(out=Oacc, in0=Oacc, in1=ong[:, None, :].broadcast_to([128, 18, 128]), op=ALU.mult), reads=["Oacc", "ong"], writes=["Oacc"])
                for tl in tiles4:
                    ps, pk = bank()
                    for jj, ti in enumerate(tl):
                        for k in range(8):
                            P.pe(lambda e: e.matmul(ps[:, jj * 128:(jj + 1) * 128], lhsT=HT[:, k, ti * 128:(ti + 1) * 128], rhs=whq[:, k, 384:512], start=(k == 0), stop=(k == 7)),
                                 reads=["HT%d" % k, "whq3"], writes=[pk])
                    P.act(lambda e: e.activation(out=SQc[:, tl[0]:tl[0] + len(tl), :], in_=ps[:, 0:128 * len(tl)].rearrange("p (a b) -> p a b", b=128), func=AF.Silu),
                          reads=[pk], writes=["SQc"])
                P.dve(lambda e: e.tensor_tensor(out=ytm, in0=Oacc, in1=SQc, op=ALU.mult), reads=["Oacc", "SQc"], writes=["ytm"])
                for tl in tiles4:
                    ps, pk = bank()
                    for jj, ti in enumerate(tl):
                        P.pe(lambda e: e.matmul(ps[:, jj * 128:(jj + 1) * 128], lhsT=ytm[:, ti, :], rhs=identb[:], start=True, stop=True), reads=["ytm", "identb"], writes=[pk])
                    P.act(lambda e: e.activation(out=yT[:, tl[0] * 128:(tl[0] + len(tl)) * 128], in_=ps[:, 0:128 * len(tl)], func=AF.Identity), reads=[pk], writes=["vT"])
                if _DBG.get("stop_C") and h == _DBG.get("dbg_head", 0):
                    P.dve(lambda e: e.tensor_copy(out=XT[:, 0, :], in_=ytm.rearrange("p a b -> p (a b)")), reads=["ytm"], writes=["XT"])
                    P.dve(lambda e: e.tensor_copy(out=XT[:, 1, :], in_=yT), reads=["vT"], writes=["XT"])
                    P.dve(lambda e: e.tensor_copy(out=XT[:, 2, :], in_=SQc.rearrange("p a b -> p (a b)")), reads=["SQc"], writes=["XT"])
                    P.dve(lambda e: e.tensor_copy(out=XT[:, 3, :], in_=Oacc.rearrange("p a b -> p (a b)")), reads=["Oacc"], writes=["XT"])
                    return
                for m in range(8):
                    for col_, seg in ((1, [(0, 256)]), (0, blocks_of(256, 2048, 512))):
                        def ev(ps, pk, s, n):
                            P.dve(lambda e: e.scalar_tensor_tensor(out=XT[:, m, s:s + n], in0=ps[:, 0:n], scalar=gate_ap(0, m, col_),
                                                                   in1=XT[:, m, s:s + n], op0=ALU.mult, op1=ALU.add), reads=[pk, "XT", "macc"], writes=["XT"])
                        linear([woh[:, m * 128:(m + 1) * 128]], [lambda s, n: yT[:, s:s + n]], seg, ev, ["woh", "vT"])

        def ffn(l, tok_groups, moe):
            p = "l%d_" % l
            P.barrier()
            off = 0
            RS, off = carve(off, [NT], F32)
            nf = 28 if moe else 22
            ne = 8 if moe else 1
            fdim = nf * 128
            if moe:
                GOFF = ARB - 6 * 1024
                LG, off2 = carve(GOFF, [18, 8], F32)
                rw, off2 = carve(off2, [8, 8], F32)
                P.dma("sp", "rw", lambda e: e.dma_start(out=rw, in_=D[p + "router"].rearrange("(k p) n -> p k n", p=128)), writes=["rw"])

                def h32_cb(k, t, tk):
                    ps, pk = bank()
                    for ti in range(18):
                        P.pe(lambda e, ps=ps, ti=ti, t=t, k=k: e.matmul(ps[:, ti * 8:(ti + 1) * 8], lhsT=t[:, ti * 128:(ti + 1) * 128], rhs=rw[:, k, :],
                                                                     start=True, stop=True), reads=[tk, "rw"], writes=[pk])
                    lgf = LG.rearrange("p a b -> p (a b)")
                    if k == 0:
                        P.dve(lambda e, ps=ps: e.tensor_copy(out=lgf, in_=ps[:, 0:144]), reads=[pk], writes=["LG"])
                    else:
                        P.dve(lambda e, ps=ps: e.tensor_tensor(out=lgf, in0=lgf, in1=ps[:, 0:144], op=ALU.add), reads=[pk, "LG"], writes=["LG"])
                norm_mod(1, 0, NT * 4, h32_cb)
                o3 = off2
                M1, o3 = carve(o3, [18], F32); M2, o3 = carve(o3, [18], F32)
                E1, o3 = carve(o3, [18, 8], F32); E2, o3 = carve(o3, [18, 8], F32); L2, o3 = carve(o3, [18, 8], F32)
                GT, o3 = carve(o3, [18, 8], F32)
                DG, o3 = carve(o3, [128], F32)
                bc = lambda a: a[:, :, None].broadcast_to([128, 18, 8])
                P.dve(lambda e: e.tensor_reduce(out=M1, in_=LG, axis=AX.X, op=ALU.max), reads=["LG"], writes=["M1"])
                P.dve(lambda e: e.tensor_tensor(out=E1, in0=LG, in1=bc(M1), op=ALU.is_equal), reads=["LG", "M1"], writes=["E1"])
                P.dve(lambda e: e.scalar_tensor_tensor(out=L2, in0=E1, scalar=-1e30, in1=LG, op0=ALU.mult, op1=ALU.add), reads=["E1", "LG"], writes=["L2"])
                P.dve(lambda e: e.tensor_reduce(out=M2, in_=L2, axis=AX.X, op=ALU.max), reads=["L2"], writes=["M2"])
                P.dve(lambda e: e.tensor_tensor(out=E2, in0=L2, in1=bc(M2), op=ALU.is_equal), reads=["L2", "M2"], writes=["E2"])
                P.dve(lambda e: e.tensor_tensor(out=M2, in0=M2, in1=M1, op=ALU.subtract), reads=["M1", "M2"], writes=["M2"])
                P.act(lambda e: e.activation(out=M2, in_=M2, func=AF.Exp), reads=["M2"], writes=["M2"])
                P.dve(lambda e: e.tensor_scalar(out=M1, in0=M2, scalar1=1.0, scalar2=None, op0=ALU.add), reads=["M2"], writes=["M1"])
                P.dve(lambda e: e.reciprocal(out=M1, in_=M1), reads=["M1"], writes=["M1"])
                P.dve(lambda e: e.tensor_tensor(out=M2, in0=M2, in1=M1, op=ALU.mult), reads=["M1", "M2"], writes=["M2"])
                P.dve(lambda e: e.tensor_tensor(out=E1, in0=E1, in1=bc(M1), op=ALU.mult), reads=["E1", "M1"], writes=["E1"])
                P.dve(lambda e: e.tensor_tensor(out=E2, in0=E2, in1=bc(M2), op=ALU.mult), reads=["E2", "M2"], writes=["E2"])
                P.dve(lambda e: e.tensor_tensor(out=GT, in0=E1, in1=E2, op=ALU.add), reads=["E1", "E2"], writes=["GT"])
                a0 = NT * 4
                alim = GOFF
            else:
                norm_mod(1, 0, NT * 4)
                a0 = NT * 4
                alim = ARB
            P.barrier()
            gmax = max(n for (_, n, _) in tok_groups)
            ACT_, a0 = carve(a0, [nf, gmax], BF16)
            GE, a0 = carve(a0, [gmax], F32)
            TS, a0 = carve(a0, [512], F32)
            TU, a0 = carve(a0, [512], F32)
            w1b = []; w2b = []
            for i in range(2):
                t, a0 = carve(a0, [8, 256], BF16); w1b.append(t)
            for i in range(2):
                t, a0 = carve(a0, [nf, 128], BF16); w2b.append(t)
            assert a0 <= alim, (a0, alim)
            cnt1 = [0]; cnt2 = [0]
            for (gs, gn, blks) in tok_groups:
                for ex in range(ne):
                    if moe:
                        w1v = D[p + "moe_w1"][ex].rearrange("(k p) n -> p k n", p=128)
                        w2v = D[p + "moe_w2"][ex]
                        for ti in range(gs // 128, (gs + gn) // 128):
                            P.dve(lambda e, ti=ti, ex=ex: e.tensor_scalar(out=DG, in0=identf[:], scalar1=GT[:, ti, ex:ex + 1], scalar2=None, op0=ALU.mult),
                                  reads=["GT", "identf"], writes=["DG"])
                            ps, pk = bank()
                            P.pe(lambda e, ps=ps: e.matmul(ps[:, 0:128], lhsT=onesf[:], rhs=DG, start=True, stop=True), reads=["DG", "onesf"], writes=[pk])
                            P.act(lambda e, ps=ps, ti=ti: e.activation(out=GE[:, ti * 128 - gs:(ti + 1) * 128 - gs], in_=ps[:, 0:128], func=AF.Identity),
                                  reads=[pk], writes=["GE"])
                    else:
                        w1v = D[p + "ffn_w1"].rearrange("(k p) n -> p k n", p=128)
                        w2v = D[p + "ffn_w2"]
                    for f in range(nf):
                        wb = w1b[cnt1[0] % 2]; wk = "w1b%d" % (cnt1[0] % 2); cnt1[0] += 1
                        P.dma("pool", wk + "g", lambda e, wb=wb, f=f, w1v=w1v: e.dma_start(out=wb[:, :, 0:128], in_=w1v[:, :, f * 128:(f + 1) * 128]), writes=[wk + "g"])
                        P.dma("pool", wk + "u", lambda e, wb=wb, f=f, w1v=w1v: e.dma_start(out=wb[:, :, 128:256], in_=w1v[:, :, fdim + f * 128:fdim + (f + 1) * 128]), writes=[wk + "u"])
                        for (s, n) in blks:
                            psg, pgk = bank()
                            psu, puk = bank()
                            for k in range(8):
                                P.pe(lambda e, psg=psg, k=k, wb=wb, s=s, n=n: e.matmul(psg[:, 0:n], lhsT=wb[:, k, 0:128], rhs=HT[:, k, s:s + n], start=(k == 0), stop=(k == 7)),
                                     reads=[wk + "g", "HT%d" % k], writes=[pgk])
                            for k in range(8):
                                P.pe(lambda e, psu=psu, k=k, wb=wb, s=s, n=n: e.matmul(psu[:, 0:n], lhsT=wb[:, k, 128:256], rhs=HT[:, k, s:s + n], start=(k == 0), stop=(k == 7)),
                                     reads=[wk + "u", "HT%d" % k], writes=[puk])
                            P.act(lambda e, psg=psg, n=n: e.activation(out=TS[:, 0:n], in_=psg[:, 0:n], func=AF.Silu), reads=[pgk], writes=["TS"])
                            if moe:
                                P.dve(lambda e, psu=psu, s=s, n=n: e.tensor_tensor(out=TU[:, 0:n], in0=psu[:, 0:n], in1=GE[:, s - gs:s - gs + n], op=ALU.mult),
                                      reads=[puk, "GE"], writes=["TU"])
                                P.dve(lambda e, f=f, s=s, n=n: e.tensor_tensor(out=ACT_[:, f, s - gs:s - gs + n], in0=TS[:, 0:n], in1=TU[:, 0:n], op=ALU.mult),
                                      reads=["TS", "TU"], writes=["ACT"])
                            else:
                                P.dve(lambda e, psu=psu, f=f, s=s, n=n: e.tensor_tensor(out=ACT_[:, f, s - gs:s - gs + n], in0=psu[:, 0:n], in1=TS[:, 0:n], op=ALU.mult),
                                      reads=[puk, "TS"], writes=["ACT"])
                    for m in range(8):
                        w2 = w2b[cnt2[0] % 2]; w2k = "w2b%d" % (cnt2[0] % 2); cnt2[0] += 1
                        P.dma("pool", w2k, lambda e, w2=w2, m=m, w2v=w2v: e.dma_start(out=w2, in_=w2v[:, m * 128:(m + 1) * 128].rearrange("(f p) n -> p f n", p=128)), writes=[w2k])
                        for (s, n) in blks:
                            ps, pk = bank()
                            for f in range(nf):
                                P.pe(lambda e, ps=ps, f=f, w2=w2, s=s, n=n: e.matmul(ps[:, 0:n], lhsT=w2[:, f, :], rhs=ACT_[:, f, s - gs:s - gs + n], start=(f == 0), stop=(f == nf - 1)),
                                     reads=[w2k, "ACT"], writes=[pk])
                            col = 1 if s < NCTX else 0
                            assert not (s < NCTX < s + n)
                            P.dve(lambda e, ps=ps, m=m, s=s, n=n, col=col: e.scalar_tensor_tensor(out=XT[:, m, s:s + n], in0=ps[:, 0:n], scalar=gate_ap(1, m, col),
                                                                                           in1=XT[:, m, s:s + n], op0=ALU.mult, op1=ALU.add),
                                  reads=[pk, "XT", "macc"], writes=["XT"])

        for l in layers:
            adaln(l)
            if l % 2 == 0:
                if not _DBG.get("skip_mix"):
                    even_mixer(l)
                if not _DBG.get("skip_ffn"):
                    ffn(l, [(0, 1280, [(0, 256), (256, 512), (768, 512)]), (1280, 1024, [(1280, 512), (1792, 512)])], moe=False)
            else:
                if not _DBG.get("skip_mix"):
                    try:
                        odd_mixer(l)
                    except _Stop:
                        pass
                if _DBG.get("skip_ffn"):
                    pass
                elif l == 3:
                    ffn(l, [(256, 768, [(256, 384), (640, 384)]), (1024, 768, [(1024, 384), (1408, 384)]),
                            (1792, 512, [(1792, 256), (2048, 256)])], moe=True)
                else:
                    ffn(l, [(0, 768, [(0, 256), (256, 512)]), (768, 768, [(768, 384), (1152, 384)]),
                            (1536, 768, [(1536, 384), (1920, 384)])], moe=True)
        P.barrier()
        if final:
            fg = smallp[:, 32:40]
            P.dma("sp", "fg", lambda e: e.dma_start(out=fg, in_=D["final_g"][:, :]), writes=["fg"])
            P.dve(lambda e: e.memset(AB[:, 0, :, :], 0.0), writes=["AB"])
            P.dve(lambda e: e.tensor_copy(out=AB[:, 0, :, 0], in_=fg), reads=["fg"], writes=["AB"])
            P.dve(lambda e: e.memset(AB[:, 1, :, :], 0.0), writes=["AB"])
            RS, _ = carve(0, [NT], F32)
            T0, _ = carve(NT * 4, [NT], F32)
            blks = ALLB
            bks = [bank() for _ in blks]
            for k in range(8):
                P.act(lambda e, k=k: e.activation(out=T0, in_=XT[:, k, :], func=AF.Square), reads=["XT"], writes=["T0"])
                for (ps, pk), (s, n) in zip(bks, blks):
                    P.pe(lambda e, ps=ps, s=s, n=n, k=k: e.matmul(ps[:, 0:n], lhsT=onesf[:], rhs=T0[:, s:s + n], start=(k == 0), stop=(k == 7)),
                         reads=["T0", "onesf"], writes=[pk])
            for (ps, pk), (s, n) in zip(bks, blks):
                P.act(lambda e, ps=ps, s=s, n=n: e.activation(out=RS[:, s:s + n], in_=ps[:, 0:n], func=AF.Sqrt, scale=1.0 / 1024, bias=epsb[:, 0:1]),
                      reads=[pk, "epsb"], writes=["RS"])
            P.dve(lambda e: e.reciprocal(out=RS, in_=RS), reads=["RS"], writes=["RS"])
            for k in range(8):
                P.dve(lambda e, k=k: e.scalar_tensor_tensor(out=XT[:, k, :], in0=XT[:, k, :], scalar=fg[:, k:k + 1], in1=RS, op0=ALU.mult, op1=ALU.mult),
                      reads=["XT", "RS", "fg"], writes=["XT"])
            tok = P.dma("sp", "out", lambda e: e.dma_start(out=out_d[:, :, :], in_=XT[:, :, NCTX:NT]), reads=["XT"])
        else:
            tok = P.dma("sp", "out", lambda e: e.dma_start(out=out_d[:, :, :], in_=XT[:]), reads=["XT"])
        P.emit(final_waits=[tok])
    return nc


def fm(v, nchunk):
    return np.ascontiguousarray(np.asarray(v, np.float32).reshape(nchunk, 128).T)


def rope_tables():
    n = 8
    inv = (10000.0 ** (-np.arange(n, dtype=np.float32) / n)).astype(np.float32)
    t = np.arange(2048)
    row = (t // 64).astype(np.float32); col = (t % 64).astype(np.float32)
    ang = np.concatenate([row[:, None] * inv, col[:, None] * inv], axis=-1).astype(np.float32)
    cos = np.cos(ang).astype(np.float32); sin = np.sin(ang).astype(np.float32)
    C = np.ones((96, NT), np.float32); S = np.zeros((96, NT), np.float32)
    cr, cc, sr, sc_ = cos[:, :8].T, cos[:, 8:].T, sin[:, :8].T, sin[:, 8:].T
    C[64:72, NCTX:] = cr; C[72:80, NCTX:] = cr; C[80:88, NCTX:] = cc; C[88:96, NCTX:] = cc
    S[64:72, NCTX:] = -sr; S[72:80, NCTX:] = sr; S[80:88, NCTX:] = -sc_; S[88:96, NCTX:] = sc_
    return np.ascontiguousarray(np.stack([C, S], axis=1))


SWAP = np.concatenate([np.arange(8, 16), np.arange(0, 8), np.arange(24, 32), np.arange(16, 24)])


def const_inputs():
    i = np.arange(128)
    m = np.zeros((128, 6, 128), np.float32)
    m[:, 0, :] = (i[:, None] <= i[None, :])
    m[:, 1, :] = (i[:, None] >= i[None, :])
    m[:, 2, :] = (i[:, None] > i[None, :])
    m[:, 3, :] = (i[:, None] < i[None, :])
    return {"ident": np.eye(128, dtype=np.float32), "masks": m, "rope": rope_tables()}


def layer_inputs(inp, l):
    p = "l%d_" % l
    d = {}
    g = lambda n: np.asarray(inp[p + n], np.float32)
    d[p + "mod_w"] = g("mod_w")
    d[p + "mod_b"] = fm(g("mod_b"), 48)
    d[p + "n1g"] = fm(g("norm1_g"), 8)
    d[p + "n2g"] = fm(g("norm2_g"), 8)
    d[p + "w_out"] = g("w_out")
    if l % 2 == 0:
        w_in = g("w_in")
        d[p + "w_in"] = w_in
        wkr = np.zeros((1024, 2, 96), np.float32)
        wkr[:, 0, 64:] = w_in[:, 1920:1952]
        wkr[:, 1, 64:] = w_in[:, 1920:1952][:, SWAP]
        d[p + "wkr"] = wkr
        d[p + "convw"] = np.ascontiguousarray(g("conv_w").reshape(3, 4, 128).transpose(2, 1, 0))
        d[p + "qng"] = fm(g("q_norm_g"), 2)
        d[p + "kvng"] = fm(g("kv_norm_g"), 1)
        wuq = g("w_uq")
        wsw = wuq.copy().reshape(256, 8, 96)
        wsw[:, :, 64:] = wsw[:, :, 64:][:, :, SWAP]
        d[p + "w_uq"] = np.ascontiguousarray(np.stack([wuq, wsw.reshape(256, 768)], axis=1))
        wukv = g("w_ukv").reshape(128, 8, 128)
        wuk = np.zeros((128, 8, 96), np.float32)
        wuk[:, :, :64] = wukv[:, :, :64]
        d[p + "w_uk"] = wuk
        d[p + "w_uv"] = np.ascontiguousarray(wukv[:, :, 64:].reshape(128, 512))
        d[p + "ffn_w1"] = g("ffn_w1")
        d[p + "ffn_w2"] = g("ffn_w2")
    else:
        d[p + "w_in"] = g("w_in")
        d[p + "convw"] = np.ascontiguousarray(g("qkv_conv_w").reshape(3, 24, 128).transpose(2, 1, 0))
        d[p + "alog"] = np.ascontiguousarray(np.broadcast_to(g("a_log").reshape(1, 16), (128, 16)))
        d[p + "dtb"] = np.ascontiguousarray(np.broadcast_to(g("dt_bias").reshape(1, 16), (128, 16)))
        d[p + "ong"] = np.ascontiguousarray(np.broadcast_to(g("o_norm_g").reshape(1, 128), (128, 128)))
        d[p + "router"] = g("router_w")
        d[p + "moe_w1"] = g("moe_w1")
        d[p + "moe_w2"] = g("moe_w2")
    return d


def core_inputs(inp, b):
    x = np.asarray(inp["x"][b], np.float32); ctx = np.asarray(inp["ctx"][b], np.float32)
    tok = np.concatenate([ctx, x], axis=0)
    xT = np.ascontiguousarray(tok.reshape(NT, 8, 128).transpose(2, 1, 0))
    cs = np.stack([np.asarray(inp["c"][b], np.float32), np.asarray(inp["c_ctx"], np.float32)], axis=-1)
    csT = np.ascontiguousarray(cs.reshape(8, 128, 2).transpose(1, 0, 2))
    return {"xT": xT, "csT": csT}


_ALL_INPUTS = (
    "x",
    "c",
    "ctx",
    "c_ctx",
    "l0_mod_w",
    "l0_mod_b",
    "l0_norm1_g",
    "l0_w_in",
    "l0_conv_w",
    "l0_q_norm_g",
    "l0_w_uq",
    "l0_kv_norm_g",
    "l0_w_ukv",
    "l0_w_out",
    "l0_norm2_g",
    "l0_ffn_w1",
    "l0_ffn_w2",
    "l1_mod_w",
    "l1_mod_b",
    "l1_norm1_g",
    "l1_w_in",
    "l1_qkv_conv_w",
    "l1_a_log",
    "l1_dt_bias",
    "l1_o_norm_g",
    "l1_w_out",
    "l1_norm2_g",
    "l1_router_w",
    "l1_moe_w1",
    "l1_moe_w2",
    "l2_mod_w",
    "l2_mod_b",
    "l2_norm1_g",
    "l2_w_in",
    "l2_conv_w",
    "l2_q_norm_g",
    "l2_w_uq",
    "l2_kv_norm_g",
    "l2_w_ukv",
    "l2_w_out",
    "l2_norm2_g",
    "l2_ffn_w1",
    "l2_ffn_w2",
    "l3_mod_w",
    "l3_mod_b",
    "l3_norm1_g",
    "l3_w_in",
    "l3_qkv_conv_w",
    "l3_a_log",
    "l3_dt_bias",
    "l3_o_norm_g",
    "l3_w_out",
    "l3_norm2_g",
    "l3_router_w",
    "l3_moe_w1",
    "l3_moe_w2",
    "final_norm_g",
)


_NC = {}


def kernel(**inputs):
    inputs = {n: inputs[n] for n in _ALL_INPUTS}
    layers = (0, 1, 2, 3)
    if "prog" not in _NC:
        _NC["prog"] = build(layers, final=True)
    nc = _NC["prog"]
    shared = const_inputs()
    shared["final_g"] = fm(inputs["final_norm_g"], 8)
    for l in layers:
        shared.update(layer_inputs(inputs, l))
    in_maps = []
    for b in range(8):
        d = dict(shared)
        d.update(core_inputs(inputs, b))
        in_maps.append(d)
    res = run_bass_kernel_spmd(nc, in_maps, core_ids=list(range(8)))
    out = np.empty((8, 2048, 1024), np.float32)
    for b in range(8):
        o = res.results[b]["outT"]
        out[b] = o.transpose(2, 1, 0).reshape(2048, 1024)
    return out
```
